# Optimizing a Trainium2 kernel written in Bass

```python
import jax, jax.numpy as jnp
from jax import lax
import numpy as np

D_MODEL = 4096
BATCH = 2
SEQ = 4096
DEPTH = 2

N_BRANCH = 4
BRANCH_WIDTH = D_MODEL // N_BRANCH
CONV_WIDTH = 31
CONV_PAD = CONV_WIDTH // 2
POOL_WINDOWS = (2, 4, 8, 16)
N_POOL_GROUPS = len(POOL_WINDOWS)
POOL_GROUP = BRANCH_WIDTH // N_POOL_GROUPS
N_FFT_GROUPS = 4
FFT_GROUP = BRANCH_WIDTH // N_FFT_GROUPS
GMLP_CHUNK = 128
GMLP_HEAD_DIM = 128
GMLP_HEADS = BRANCH_WIDTH // GMLP_HEAD_DIM
IN_COLS = 6 * BRANCH_WIDTH
D_FF = 256 * ((8 * D_MODEL // 3 + 255) // 256)
N_EXPERTS = 8
TOP_K = 2
D_FF_EXPERT = D_MODEL
EPS = 1e-6

kernel_name = 'hybrid_conv_pool_fourier_gmlp_moe_encoder'


def rmsnorm(x, g):
    xf = x.astype(jnp.float32)
    y = xf * lax.rsqrt(jnp.mean(xf * xf, axis=-1, keepdims=True) + EPS)
    return y.astype(x.dtype) * g


def layernorm(x, g, b):
    xf = x.astype(jnp.float32)
    mu = jnp.mean(xf, axis=-1, keepdims=True)
    var = jnp.mean(jnp.square(xf - mu), axis=-1, keepdims=True)
    return ((xf - mu) * lax.rsqrt(var + EPS)).astype(x.dtype) * g + b


def conv_branch(a_lin, a_gate, conv_w, conv_b, ln_g, ln_b):
    a = a_lin * jax.nn.sigmoid(a_gate)
    y = lax.conv_general_dilated(a, conv_w[:, None, :], (1,), [(CONV_PAD, CONV_PAD)],
                                 dimension_numbers=('NWC', 'WIO', 'NWC'),
                                 feature_group_count=BRANCH_WIDTH) + conv_b
    return jax.nn.silu(layernorm(y, ln_g, ln_b))


def pool_branch(p, pool_w, pool_scale):
    B, S, _ = p.shape
    pf = p.astype(jnp.float32).reshape(B, S, N_POOL_GROUPS, POOL_GROUP)
    cs = jnp.concatenate([jnp.zeros((B, 1, N_POOL_GROUPS, POOL_GROUP), jnp.float32),
                          jnp.cumsum(pf, axis=1)], axis=1)
    t = jnp.arange(S)
    outs = []
    for g, w in enumerate(POOL_WINDOWS):
        lo = jnp.clip(t - w // 2, 0, S - 1)
        hi = jnp.clip(t + w // 2 - 1, 0, S - 1)
        win_sum = cs[:, hi + 1, g] - cs[:, lo, g]
        cnt = (hi - lo + 1).astype(jnp.float32)[None, :, None]
        outs.append(win_sum / cnt - pf[:, :, g])
    pooled = jnp.stack(outs, axis=2).astype(p.dtype)
    mixed = jnp.einsum('bsgc,gce->bsge', pooled, pool_w)
    return mixed.reshape(B, S, BRANCH_WIDTH) * pool_scale


def fourier_branch(f):
    B, S, _ = f.shape
    fg = f.astype(jnp.float32).reshape(B, S, N_FFT_GROUPS, FFT_GROUP)
    y = jnp.fft.fft2(fg, axes=(1, 3), norm='ortho').real
    return y.astype(f.dtype).reshape(B, S, BRANCH_WIDTH)


def gmlp_branch(u, v, ln_g, ln_b, ws, bias):
    B, S, _ = v.shape
    vn = layernorm(v, ln_g, ln_b).reshape(B, S // GMLP_CHUNK, GMLP_CHUNK, GMLP_HEADS, GMLP_HEAD_DIM)
    s = jnp.einsum('hpq,bnqhc->bnphc', ws, vn) + bias.T[None, None, :, :, None]
    return u * s.reshape(B, S, BRANCH_WIDTH)


def hybrid_mixer(xn, w_in, w_gate, conv_w, conv_b, conv_ln_g, conv_ln_b, pool_w, pool_scale,
                 gmlp_ln_g, gmlp_ln_b, gmlp_ws, gmlp_b, w_branch, w_out):
    B, S, _ = xn.shape
    h = xn @ w_in
    a_lin, a_gate, p_in, f_in, u, v = jnp.split(h, 6, axis=-1)
    ya = conv_branch(a_lin, a_gate, conv_w, conv_b, conv_ln_g, conv_ln_b)
    yb = pool_branch(p_in, pool_w, pool_scale)
    yc = fourier_branch(f_in)
    yd = gmlp_branch(u, v, gmlp_ln_g, gmlp_ln_b, gmlp_ws, gmlp_b)
    branches = jnp.stack([ya, yb, yc, yd], axis=2)
    proj = jnp.einsum('bsgc,gcd->bsgd', branches, w_branch)
    gates = jax.nn.sigmoid((xn @ w_gate).reshape(B, S, N_BRANCH, D_MODEL))
    merged = jnp.einsum('bsgd,bsgd->bsd', gates, proj)
    return merged @ w_out


def swiglu(x, w1, w3, w2):
    return (jax.nn.silu(x @ w1) * (x @ w3)) @ w2


def moe_swiglu(xn, router, w1, w3, w2):
    B, S, D = xn.shape
    t = xn.reshape(B * S, D)
    logits = (t @ router).astype(jnp.float32)
    top_v, top_i = lax.top_k(logits, TOP_K)
    top_w = jax.nn.softmax(top_v, axis=-1)
    combine = jnp.sum(jax.nn.one_hot(top_i, N_EXPERTS, dtype=jnp.float32) * top_w[..., None],
                      axis=1).astype(t.dtype)
    y = jnp.zeros_like(t)
    for e in range(N_EXPERTS):
        y = y + combine[:, e:e + 1] * swiglu(t, w1[e], w3[e], w2[e])
    return y.reshape(B, S, D)


def setup_inputs(seed: int = 0) -> dict:
    key = jax.random.key(seed)
    keys = iter(jax.random.split(key, 128))
    f32 = jnp.float32

    def nrm(shape, scale):
        return jax.random.normal(next(keys), shape, f32) * scale

    def gain(n):
        return 1.0 + nrm((n,), 0.02)

    D, BW = D_MODEL, BRANCH_WIDTH
    out = {'x': nrm((BATCH, SEQ, D), 1.0)}
    for i in range(DEPTH):
        p = 'l%d_' % i
        out[p + 'norm1'] = gain(D)
        out[p + 'w_in'] = nrm((D, IN_COLS), D ** -0.5)
        out[p + 'w_gate'] = nrm((D, N_BRANCH * D), D ** -0.5)
        out[p + 'conv_w'] = nrm((CONV_WIDTH, BW), CONV_WIDTH ** -0.5)
        out[p + 'conv_b'] = nrm((BW,), 0.01)
        out[p + 'conv_ln_g'] = gain(BW)
        out[p + 'conv_ln_b'] = nrm((BW,), 0.01)
        out[p + 'pool_w'] = nrm((N_POOL_GROUPS, POOL_GROUP, POOL_GROUP), POOL_GROUP ** -0.5)
        out[p + 'pool_scale'] = gain(BW)
        out[p + 'gmlp_ln_g'] = gain(BW)
        out[p + 'gmlp_ln_b'] = nrm((BW,), 0.01)
        out[p + 'gmlp_ws'] = nrm((GMLP_HEADS, GMLP_CHUNK, GMLP_CHUNK), GMLP_CHUNK ** -0.5)
        out[p + 'gmlp_b'] = 1.0 + nrm((GMLP_HEADS, GMLP_CHUNK), 0.01)
        out[p + 'w_branch'] = nrm((N_BRANCH, BW, D), BW ** -0.5)
        out[p + 'w_out'] = nrm((D, D), D ** -0.5)
        out[p + 'norm2'] = gain(D)
        if i % 2 == 0:
            out[p + 'ffn_w1'] = nrm((D, D_FF), D ** -0.5)
            out[p + 'ffn_w3'] = nrm((D, D_FF), D ** -0.5)
            out[p + 'ffn_w2'] = nrm((D_FF, D), D_FF ** -0.5)
        else:
            out[p + 'router'] = nrm((D, N_EXPERTS), D ** -0.5)
            out[p + 'exp_w1'] = nrm((N_EXPERTS, D, D_FF_EXPERT), D ** -0.5)
            out[p + 'exp_w3'] = nrm((N_EXPERTS, D, D_FF_EXPERT), D ** -0.5)
            out[p + 'exp_w2'] = nrm((N_EXPERTS, D_FF_EXPERT, D), D_FF_EXPERT ** -0.5)
    out['final_norm'] = gain(D)
    return out


def reference(x,
              l0_norm1, l0_w_in, l0_w_gate, l0_conv_w, l0_conv_b, l0_conv_ln_g, l0_conv_ln_b,
              l0_pool_w, l0_pool_scale, l0_gmlp_ln_g, l0_gmlp_ln_b, l0_gmlp_ws, l0_gmlp_b,
              l0_w_branch, l0_w_out, l0_norm2, l0_ffn_w1, l0_ffn_w3, l0_ffn_w2,
              l1_norm1, l1_w_in, l1_w_gate, l1_conv_w, l1_conv_b, l1_conv_ln_g, l1_conv_ln_b,
              l1_pool_w, l1_pool_scale, l1_gmlp_ln_g, l1_gmlp_ln_b, l1_gmlp_ws, l1_gmlp_b,
              l1_w_branch, l1_w_out, l1_norm2, l1_router, l1_exp_w1, l1_exp_w3, l1_exp_w2,
              final_norm):
    norm1 = [l0_norm1, l1_norm1]
    mixer_params = [
        (l0_w_in, l0_w_gate, l0_conv_w, l0_conv_b, l0_conv_ln_g, l0_conv_ln_b, l0_pool_w,
         l0_pool_scale, l0_gmlp_ln_g, l0_gmlp_ln_b, l0_gmlp_ws, l0_gmlp_b, l0_w_branch, l0_w_out),
        (l1_w_in, l1_w_gate, l1_conv_w, l1_conv_b, l1_conv_ln_g, l1_conv_ln_b, l1_pool_w,
         l1_pool_scale, l1_gmlp_ln_g, l1_gmlp_ln_b, l1_gmlp_ws, l1_gmlp_b, l1_w_branch, l1_w_out),
    ]
    norm2 = [l0_norm2, l1_norm2]
    ffn_params = [(l0_ffn_w1, l0_ffn_w3, l0_ffn_w2),
                  (l1_router, l1_exp_w1, l1_exp_w3, l1_exp_w2)]
    h = x
    for i in range(DEPTH):
        h = h + hybrid_mixer(rmsnorm(h, norm1[i]), *mixer_params[i])
        hn = rmsnorm(h, norm2[i])
        if i % 2 == 0:
            h = h + swiglu(hn, *ffn_params[i])
        else:
            h = h + moe_swiglu(hn, *ffn_params[i])
    return rmsnorm(h, final_norm)
```

```python
import numpy as np
from contextlib import ExitStack
import ml_dtypes
import concourse.bass as bass
import concourse.mybir as mybir
from concourse.bass_utils import run_bass_kernel_spmd

F32 = mybir.dt.float32
BF16 = mybir.dt.bfloat16
U8 = mybir.dt.uint8
AF = mybir.ActivationFunctionType
ALU = mybir.AluOpType
AX = mybir.AxisListType

NCORES = 2
NBLK = 4
D = 4096
T = 1024
S = 4096
BW = 1024
DFF = 11008
NE = 8
EPS = 1e-6
ND = 8
SB_BYTES = 206 * 1024


class Res:
    __slots__ = ("name", "w", "r", "epoch", "persist", "pend")

    def __init__(self, name="", persist=False):
        self.name = name
        self.w = []
        self.r = []
        self.epoch = 0
        self.persist = persist
        self.pend = None


class KB:
    def __init__(self, nc, st):
        self.nc = nc
        self.engs = {"pe": nc.tensor, "act": nc.scalar, "dve": nc.vector, "pool": nc.gpsimd, "sp": nc.sync}
        self.csem = {}
        self.ccnt = {}
        for e in ("pe", "act", "dve", "pool"):
            self.csem[e] = st.enter_context(nc.semaphore("c_" + e))
            self.ccnt[e] = 0
        self.dsem = {}
        self.dcnt = {}
        self.dnext = {}
        for q in ("sp", "act", "pool"):
            self.dsem[q] = [st.enter_context(nc.semaphore("d_%s%d" % (q, i))) for i in range(ND)]
            self.dcnt[q] = [0] * ND
            self.dnext[q] = 0
        self.ccsem = st.enter_context(nc.semaphore("ccs"))
        self.cccnt = 0
        self.known = {e: {} for e in self.engs}
        self.stream = {e: [] for e in self.engs}
        self.pending = {e: [] for e in self.engs}
        self.epoch = 1
        self.ninst = 0
        self.arena = st.enter_context(nc.sbuf_tensor("arena", [128, SB_BYTES], U8))
        self.sb_off = 0
        self.sb_base = 0
        self.psum = [st.enter_context(nc.psum_tensor("ps%d" % i, [128, 512], F32)) for i in range(8)]
        self.psres = [Res("ps%d" % i) for i in range(8)]

    def sb(self, shape, dtype, persist=False):
        esz = 4 if dtype == F32 else (2 if dtype == BF16 else 1)
        n = 1
        for s_ in shape:
            n *= s_
        nbytes = (n * esz + 63) // 64 * 64
        off = self.sb_off
        self.sb_off += nbytes
        assert self.sb_off <= SB_BYTES, "SBUF overflow %d" % self.sb_off
        ap = self.arena[:, off:off + n * esz]
        if dtype != U8:
            ap = ap.bitcast(dtype)
        if len(shape) == 2:
            ap = ap.rearrange("p (a b) -> p a b", b=shape[1])
        elif len(shape) == 3:
            ap = ap.rearrange("p (a b c) -> p a b c", b=shape[1], c=shape[2])
        if persist:
            self.sb_base = self.sb_off
        return ap

    def ps(self, i, dtype=F32):
        ap = self.psum[i][:, :]
        if dtype == BF16:
            ap = ap.bitcast(BF16)
        return ap

    def _touch(self, r):
        if r.epoch != self.epoch and not r.persist:
            r.w = []
            r.r = []
            r.pend = None
        r.epoch = self.epoch

    def _deps(self, e, reads, writes, join=False):
        evs = []
        for r in reads:
            self._touch(r)
            assert r.pend is None or r.pend == e, "pending unsignaled access on %s by %s" % (r.name, r.pend)
            evs.extend(r.w)
        for w in writes:
            self._touch(w)
            assert w.pend is None or w.pend == e, "pending unsignaled access on %s by %s" % (w.name, w.pend)
            if not join:
                evs.extend(w.w)
            evs.extend(w.r)
        kn = self.known[e]
        best = {}
        for (sem, val, owner) in evs:
            if owner == e and e == "pe":
                continue
            k = id(sem)
            if kn.get(k, 0) >= val:
                continue
            if k not in best or best[k][1] < val:
                best[k] = (sem, val)
        out = []
        for k, (sem, val) in best.items():
            kn[k] = val
            out.append((sem, val))
        return out

    def _attach(self, ev, reads, writes, join=False):
        for r in reads:
            r.r = [x for x in r.r if x[0] is not ev[0]] + [ev]
            r.pend = None
        for w in writes:
            if join:
                w.w = w.w + [ev]
            else:
                w.w = [ev]
            w.r = []
            w.pend = None

    def op(self, e, fn, reads=(), writes=(), signal=True):
        reads = list(reads)
        writes = list(writes)
        waits = self._deps(e, reads, writes)
        sem = self.csem[e]
        if signal:
            self.ccnt[e] += 1
            ev = (sem, self.ccnt[e], e)
            pr = [x for x, k in self.pending[e] if k == "r"]
            pw = [x for x, k in self.pending[e] if k == "w"]
            self.pending[e] = []
            self._attach(ev, pr + reads, pw + writes)
        else:
            for r in reads:
                r.pend = e
                self.pending[e].append((r, "r"))
            for w in writes:
                w.pend = e
                self.pending[e].append((w, "w"))
        self.ninst += 1

        def thunk(eng):
            for s_, v in waits:
                eng.wait_ge(s_, v)
            ins = fn(eng)
            if signal:
                ins.then_inc(sem, 1)
        self.stream[e].append(thunk)

    def dma(self, q, out, in_, reads=(), writes=(), join=False, **kw):
        reads = list(reads)
        writes = list(writes)
        i = self.dnext[q]
        self.dnext[q] = (i + 1) % ND
        sem = self.dsem[q][i]
        prev = self.dcnt[q][i]
        waits = self._deps(q, reads, writes, join=join)
        if prev > 0 and self.known[q].get(id(sem), 0) < prev:
            waits.append((sem, prev))
            self.known[q][id(sem)] = prev
        self.dcnt[q][i] = prev + 16
        ev = (sem, prev + 16, "dma")
        self._attach(ev, reads, writes, join=join)
        self.ninst += 1

        def thunk(eng):
            for s_, v in waits:
                eng.wait_ge(s_, v)
            eng.dma_start(out=out, in_=in_, **kw).then_inc(sem, 16)
        self.stream[q].append(thunk)

    def cc(self, kind, ins, outs, groups, reads=(), writes=()):
        reads = list(reads)
        writes = list(writes)
        waits = self._deps("pool", reads, writes)
        if self.cccnt > 0 and self.known["pool"].get(id(self.ccsem), 0) < self.cccnt:
            waits.append((self.ccsem, self.cccnt))
            self.known["pool"][id(self.ccsem)] = self.cccnt
        self.cccnt += 1
        sem = self.ccsem
        ev = (sem, self.cccnt, "cc")
        self._attach(ev, reads, writes)

        def thunk(eng):
            for s_, v in waits:
                eng.wait_ge(s_, v)
            eng.collective_compute(kind, ALU.bypass, replica_groups=groups, ins=ins, outs=outs).then_inc(sem, 1)
        self.stream["pool"].append(thunk)

    def barrier(self):
        for e in self.engs:
            assert not self.pending[e], "pending at barrier on " + e
        targets = []
        for e in ("pe", "act", "dve"):
            if self.ccnt[e] > 0:
                targets.append((self.csem[e], self.ccnt[e]))
        for q in ("sp", "act"):
            for i in range(ND):
                if self.dcnt[q][i] > 0:
                    targets.append((self.dsem[q][i], self.dcnt[q][i]))
        for e in ("pe", "act", "dve", "sp"):
            kn = self.known[e]
            waits = []
            for sem, v in targets:
                if kn.get(id(sem), 0) < v:
                    kn[id(sem)] = v
                    waits.append((sem, v))

            def thunk(eng, waits=waits):
                for s_, v in waits:
                    eng.wait_ge(s_, v)
            self.stream[e].append(thunk)
        self.epoch += 1
        self.sb_off = self.sb_base

    def finish(self):
        nc = self.nc
        fin = []
        if self.cccnt > 0:
            fin.append((self.ccsem, self.cccnt))
        for i in range(ND):
            if self.dcnt["pool"][i] > 0:
                fin.append((self.dsem["pool"][i], self.dcnt["pool"][i]))
        for e in ("pool", "sp"):
            def thunk(eng, fin=fin):
                for s_, v in fin:
                    eng.wait_ge(s_, v)
            self.stream[e].append(thunk)
        with nc.Block() as block:
            for e, deco in (("sp", block.sync), ("act", block.scalar), ("dve", block.vector),
                            ("pool", block.gpsimd), ("pe", block.tensor)):
                thunks = self.stream[e]
                self.stream[e] = []

                def run(eng, thunks=thunks):
                    for t_ in thunks:
                        t_(eng)
                deco(run)

    def pool_wait_all(self):
        for e in ("pe", "act", "dve", "sp"):
            assert not self.pending[e]
        targets = []
        for e in ("pe", "act", "dve"):
            if self.ccnt[e] > 0:
                targets.append((self.csem[e], self.ccnt[e]))
        for q in ("sp", "act"):
            for i in range(ND):
                if self.dcnt[q][i] > 0:
                    targets.append((self.dsem[q][i], self.dcnt[q][i]))
        kn = self.known["pool"]
        waits = []
        for sem, v in targets:
            if kn.get(id(sem), 0) < v:
                kn[id(sem)] = v
                waits.append((sem, v))

        def thunk(eng, waits=waits):
            for s_, v in waits:
                eng.wait_ge(s_, v)
        self.stream["pool"].append(thunk)

    def mm_group(self, out_ap, bank, n, lhs_fn, rhs_fn, reads):
        for i in range(n):
            rd = reads(i) if callable(reads) else reads
            self.op("pe", lambda e, i=i: e.matmul(out_ap, lhs_fn(i), rhs_fn(i), start=(i == 0), stop=(i == n - 1)),
                    reads=rd, writes=[self.psres[bank]], signal=(i == n - 1))


class XRes:
    def __init__(self, parts):
        self.parts = parts

    def kc(self, i):
        return self.parts[i // 8]


def pipeline(loads, computes, depth=2, after_first=None):
    n = len(computes)
    loads[0]()
    if after_first is not None:
        after_first()
    for j in range(1, min(depth, n)):
        loads[j]()
    for j in range(n):
        if j + depth < n:
            loads[j + depth]()
        computes[j]()


class Ring:
    def __init__(self, k, n, shape, dtype, name):
        self.bufs = [k.sb(shape, dtype) for _ in range(n)]
        self.res = [Res("%s%d" % (name, i)) for i in range(n)]
        self.i = 0
        self.n = n

    def next(self):
        i = self.i
        self.i = (i + 1) % self.n
        return self.bufs[i], self.res[i]


def rows_p(ap, p=128):
    return ap.rearrange("(c p) n -> p c n", p=p)


class Prog:
    def __init__(self, debug=False):
        self.debug = debug
        self.nc = nc = bass.Bass("TRN2", target_bir_lowering=False)
        self.inp = {}
        self.dbg_outs = {}

    def ext(self, name, shape, dtype=F32):
        t = self.nc.dram_tensor(name, list(shape), dtype, kind="ExternalInput").ap()
        self.inp[name] = (tuple(shape), dtype)
        return t

    def dram(self, name, shape, dtype):
        return self.nc.dram_tensor(name, list(shape), dtype).ap()

    def build(self, nlayers=2, stop_after=None, need=None, nblk=NBLK):
        nc = self.nc
        P = self
        x_in = P.ext("x", [NBLK * T, D])
        out_t = nc.dram_tensor("out", [NBLK * T, D], F32, kind="ExternalOutput").ap()
        ident_in = P.ext("ident", [128, 128])
        dftab = P.ext("dftab", [NBLK * S, 2048], BF16)
        cdft = P.ext("cdft", [256, 512], BF16)
        invcnt = P.ext("invcnt", [NBLK * 4 * T])
        L = []
        for i in range(nlayers):
            p = "l%d_" % i
            d = {}

            def wext(nm, shape, p=p, d=d):
                if need is None or nm in need:
                    d[nm] = P.ext(p + nm, shape)
            d["norm1"] = P.ext(p + "norm1", [D])
            d["norm2"] = P.ext(p + "norm2", [D])
            wext("w_in", [D, 6144])
            wext("w_gate", [D, 4 * D])
            wext("w_branch", [D, D])
            wext("w_out", [D, D])
            d["conv_wT"] = P.ext(p + "conv_wT", [BW, 31])
            d["chanp"] = P.ext(p + "chanp", [128, 32])
            d["pool_w"] = P.ext(p + "pool_w", [4 * 256, 256])
            d["gmlp_ln_g"] = P.ext(p + "gmlp_ln_g", [BW])
            d["gmlp_ln_b"] = P.ext(p + "gmlp_ln_b", [BW])
            d["gmlp_wsT"] = P.ext(p + "gmlp_wsT", [128, 8 * 128])
            d["gmlp_b"] = P.ext(p + "gmlp_b", [8 * 128])
            if i % 2 == 0:
                wext("ffn_w1", [D, DFF])
                wext("ffn_w3", [D, DFF])
                wext("ffn_w2", [DFF, D])
            else:
                d["routerT"] = P.ext(p + "routerT", [NE * D])
                wext("exp_w1", [NE * D, D])
                wext("exp_w3", [NE * D, D])
                wext("exp_w2", [NE * D, D])
            L.append(d)
        final_norm = P.ext("final_norm", [D])

        B_ = range(nblk)
        h = [P.dram("h%d" % q, [T, D], F32) for q in B_]
        xnT = [P.dram("xnT%d" % q, [D, T], BF16) for q in B_]
        aT = [P.dram("aT%d" % q, [BW, T], F32) for q in B_]
        pT = [P.dram("pT%d" % q, [BW, T], F32) for q in B_]
        fT = P.dram("fT", [BW, T], BF16)
        uT = [P.dram("uT%d" % q, [BW, T], F32) for q in B_]
        vn = [P.dram("vn%d" % q, [T, BW], BF16) for q in B_]
        edges_g = P.dram("edges_g", [NBLK * BW, 64], F32)
        xcs_g = P.dram("xcs_g", [S, 2048], BF16)
        brT = P.dram("brT", [4 * BW, T], BF16)
        mergedT = P.dram("mergedT", [D, T], BF16)
        GT = P.dram("GT", [D, T], BF16)
        cmbT = P.dram("cmbT", [NE, T], F32)

        with ExitStack() as st:
            k = KB(nc, st)
            self.k = k
            identf = k.sb([128], F32, persist=True)
            identb = k.sb([128], BF16, persist=True)
            onesf = k.sb([128], F32, persist=True)
            epsb = k.sb([1], F32, persist=True)
            r_c = Res("consts", persist=True)
            self.identf, self.identb, self.onesf, self.epsb = identf, identb, onesf, epsb
            k.dma("sp", identf, ident_in, writes=[r_c])
            k.op("dve", lambda e: e.tensor_copy(out=identb, in_=identf), reads=[r_c], writes=[r_c])
            k.op("dve", lambda e: e.memset(onesf, 1.0), writes=[r_c])
            k.op("dve", lambda e: e.memset(epsb, EPS), writes=[r_c])
            k.barrier()

            W = [dict() for _ in range(nlayers)]

            def prep(li, name, kind):
                if name not in L[li]:
                    W[li][name] = None
                    return
                src = L[li][name]
                rows, cols = src.shape
                full = P.dram("wf_%d_%s" % (li, name), [rows, cols], BF16)
                r_full = Res("wf_%d_%s" % (li, name), persist=True)
                step = max(128, (8 * 1024 * 1024 // cols) // 128 * 128)
                r0 = 0
                first = True
                while r0 < rows:
                    r1 = min(rows, r0 + step)
                    k.dma("pool", full[r0:r1, :], src[r0:r1, :], writes=[r_full], join=not first)
                    first = False
                    r0 = r1
                if kind == "exp":
                    blocks = [full[e_ * D:(e_ + 1) * D, :] for e_ in range(NE)]
                else:
                    blocks = [full]
                W[li][name] = {"kind": kind, "blocks": blocks, "res": r_full, "rows_loc": rows}

            for li in range(nlayers):
                for name in ("w_in", "w_gate", "w_branch", "w_out"):
                    prep(li, name, "row")
                if li % 2 == 0:
                    for name in ("ffn_w1", "ffn_w3", "ffn_w2"):
                        prep(li, name, "row")
                else:
                    for name in ("exp_w1", "exp_w3", "exp_w2"):
                        prep(li, name, "exp")
                for nm in ("w_in", "w_gate", "w_branch", "w_out", "ffn_w1", "ffn_w3", "ffn_w2", "exp_w1", "exp_w3", "exp_w2"):
                    W[li].setdefault(nm, None)

            self.stopped = False

            def stop(tag):
                if stop_after == tag:
                    self.stopped = True
                return self.stopped

            h_src = [x_in[q * T:(q + 1) * T, :] for q in B_]
            for li in range(nlayers):
                if self.stopped:
                    break
                d = L[li]
                w = W[li]
                for q in B_:
                    self.ph_norm(h_src[q], d["norm1"], xnT[q])
                    if stop("n1"): break
                    self.ph_inproj(w["w_in"], xnT[q], aT[q], pT[q], fT, uT[q], edges_g[q * BW:(q + 1) * BW, :])
                    if stop("p2a"): break
                    self.ph_vproj(w["w_in"], xnT[q], vn[q], d["gmlp_ln_g"], d["gmlp_ln_b"])
                    self.ph_four1(fT, cdft, xcs_g[q * T:(q + 1) * T, :])
                if self.stopped: break
                if stop("p3a"): break
                for q in B_:
                    eL = edges_g[(q - 1) * BW:q * BW, :] if q > 0 else None
                    eR = edges_g[(q + 1) * BW:(q + 2) * BW, :] if q < nblk - 1 else None
                    self.ph_conv(aT[q], eL, eR, d["conv_wT"], d["chanp"], brT)
                    self.ph_pool(pT[q], eL, eR, invcnt[q * 4 * T:(q + 1) * 4 * T], d["pool_w"], d["chanp"], brT)
                    self.ph_gmlp(vn[q], uT[q], d["gmlp_wsT"], d["gmlp_b"], brT)
                    self.ph_four2(xcs_g, dftab[q * S:(q + 1) * S, :], brT)
                    if stop("p3"): break
                    self.ph_merge(w["w_gate"], w["w_branch"], xnT[q], brT, mergedT)
                    if stop("p4"): break
                    self.ph_outproj(w["w_out"], 0, 32, mergedT, h_src[q], h[q])
                    if stop("p5"): break
                    self.ph_norm(h[q], d["norm2"], xnT[q])
                    if li % 2 == 0:
                        c0 = 0
                        for nch in (32, 32, 22):
                            self.ph_ffn1(w["ffn_w1"], w["ffn_w3"], c0, nch, xnT[q], GT, None)
                            self.ph_outproj(w["ffn_w2"], c0, nch, GT, h[q], h[q])
                            c0 += nch
                    else:
                        self.ph_router(h[q], d["norm2"], d["routerT"], cmbT)
                        for e in range(NE):
                            self.ph_ffn1(w["exp_w1"], w["exp_w3"], 0, 32, xnT[q], GT, cmbT[e, :], wrow0=e * D)
                            self.ph_outproj(w["exp_w2"], e * 32, 32, GT, h[q], h[q])
                    if stop("l%d" % li): break
                h_src = h
            if not self.stopped:
                for q in B_:
                    self.ph_final(h[q], final_norm, out_t[q * T:(q + 1) * T, :])
            else:
                import os as _os
                want = _os.environ.get("DUMP", "xnT,aT,pT,uT,vn,brT").split(",")
                rr = None
                for name, ap in (("h", h[0]), ("xnT", xnT[0]), ("aT", aT[0]), ("pT", pT[0]), ("fT", fT), ("uT", uT[0]), ("vn", vn[0]),
                                 ("brT", brT), ("mergedT", mergedT), ("xcs_g", xcs_g), ("edges_g", edges_g), ("GT", GT), ("cmbT", cmbT)):
                    if name not in want:
                        continue
                    o = nc.dram_tensor("dbg_" + name, list(ap.shape), ap.dtype, kind="ExternalOutput").ap()
                    self.dbg_outs[name] = o
                    rr = Res("dbgcopy")
                    k.dma("sp", o, ap, writes=[rr])
                k.barrier()
            k.finish()
        return nc

    def _rstd(self, ss, n, tag):
        k = self.k
        sd = k.sb([n], F32)
        rstd = k.sb([n], F32)
        r_sd = Res("sd" + tag)
        r_rstd = Res("rstd" + tag)
        return sd, rstd, r_sd, r_rstd

    def ph_norm(self, h_src, gvec, xT_dst):
        k = self.k
        gbc = k.sb([D], F32); r_g = Res("gbc")
        k.dma("sp", gbc, gvec.partition_broadcast(128), writes=[r_g])
        xT = k.sb([32, T], BF16)
        r_xT = [Res("xT%d" % i) for i in range(8)]
        hring = Ring(k, 2, [D], F32, "ht")
        hsring = Ring(k, 2, [D], BF16, "hs")
        junk = k.sb([D], BF16); r_junk = Res("junk")
        ss = k.sb([8], F32); sd = k.sb([8], F32); rstd = k.sb([8], F32)
        r_ss = [Res("ss%d" % i) for i in range(8)]
        r_sd = [Res("sd%d" % i) for i in range(8)]
        r_rs = [Res("rs%d" % i) for i in range(8)]
        tiles = {}

        def load(tt):
            ht, r_ht = hring.next()
            tiles[tt] = (ht, r_ht)
            k.dma("sp", ht, h_src[tt * 128:(tt + 1) * 128, :], writes=[r_ht])

        def comp(tt):
            ht, r_ht = tiles[tt]
            hs, r_hs = hsring.next()
            k.op("act", lambda e: e.activation(out=junk, in_=ht, func=AF.Square, accum_out=ss[:, tt:tt + 1]),
                 reads=[r_ht], writes=[r_junk, r_ss[tt]])
            k.op("act", lambda e: e.activation(out=sd[:, tt:tt + 1], in_=ss[:, tt:tt + 1], func=AF.Sqrt, bias=self.epsb[:, 0:1], scale=1.0 / D),
                 reads=[r_ss[tt]], writes=[r_sd[tt]])
            k.op("dve", lambda e: e.reciprocal(out=rstd[:, tt:tt + 1], in_=sd[:, tt:tt + 1]), reads=[r_sd[tt]], writes=[r_rs[tt]])
            k.op("dve", lambda e: e.scalar_tensor_tensor(out=hs, in0=ht, scalar=rstd[:, tt:tt + 1], in1=gbc, op0=ALU.mult, op1=ALU.mult),
                 reads=[r_ht, r_rs[tt], r_g], writes=[r_hs])
            for j in range(4):
                bank = (tt % 2) * 4 + j
                pt = k.ps(bank, BF16)
                for f in range(8):
                    fc = j * 8 + f
                    k.op("pe", lambda e, f=f, fc=fc, pt=pt: e.transpose(pt[:, f * 128:(f + 1) * 128], hs[:, fc * 128:(fc + 1) * 128], self.identb),
                         reads=[r_hs], writes=[k.psres[bank]], signal=(f == 7))
                eng = "act" if j % 2 == 0 else "dve"
                dst = xT[:, j * 8:(j + 1) * 8, tt * 128:(tt + 1) * 128]
                srcv = pt.rearrange("p (a b) -> p a b", b=128)
                if eng == "act":
                    k.op("act", lambda e, dst=dst, srcv=srcv: e.activation(out=dst, in_=srcv, func=AF.Copy), reads=[k.psres[bank]], writes=[r_xT[tt]])
                else:
                    k.op("dve", lambda e, dst=dst, srcv=srcv: e.tensor_copy(out=dst, in_=srcv), reads=[k.psres[bank]], writes=[r_xT[tt]])

        pipeline([lambda tt=tt: load(tt) for tt in range(8)], [lambda tt=tt: comp(tt) for tt in range(8)], depth=1)
        k.dma("sp", rows_p(xT_dst), xT, reads=r_xT)
        k.barrier()

    def ph_final(self, h_src, gvec, out_t):
        k = self.k
        gbc = k.sb([D], F32); r_g = Res("gbc")
        k.dma("sp", gbc, gvec.partition_broadcast(128), writes=[r_g])
        hring = Ring(k, 2, [D], F32, "ht")
        oring = Ring(k, 2, [D], F32, "ot")
        junk = k.sb([D], BF16); r_junk = Res("junk")
        ss = k.sb([8], F32); sd = k.sb([8], F32); rstd = k.sb([8], F32)
        r_ss = [Res("ss%d" % i) for i in range(8)]
        r_sd = [Res("sd%d" % i) for i in range(8)]
        r_rs = [Res("rs%d" % i) for i in range(8)]
        for tt in range(8):
            ht, r_ht = hring.next()
            ot, r_ot = oring.next()
            k.dma("sp", ht, h_src[tt * 128:(tt + 1) * 128, :], writes=[r_ht])
            k.op("act", lambda e, ht=ht, tt=tt: e.activation(out=junk, in_=ht, func=AF.Square, accum_out=ss[:, tt:tt + 1]),
                 reads=[r_ht], writes=[r_junk, r_ss[tt]])
            k.op("act", lambda e, tt=tt: e.activation(out=sd[:, tt:tt + 1], in_=ss[:, tt:tt + 1], func=AF.Sqrt, bias=self.epsb[:, 0:1], scale=1.0 / D),
                 reads=[r_ss[tt]], writes=[r_sd[tt]])
            k.op("dve", lambda e, tt=tt: e.reciprocal(out=rstd[:, tt:tt + 1], in_=sd[:, tt:tt + 1]), reads=[r_sd[tt]], writes=[r_rs[tt]])
            k.op("dve", lambda e, ht=ht, ot=ot, tt=tt: e.scalar_tensor_tensor(out=ot, in0=ht, scalar=rstd[:, tt:tt + 1], in1=gbc, op0=ALU.mult, op1=ALU.mult),
                 reads=[r_ht, r_rs[tt], r_g], writes=[r_ot])
            k.dma("sp", out_t[tt * 128:(tt + 1) * 128, :], ot, reads=[r_ot])
        k.barrier()

    def _load_xT(self, src, nch=32, name="xT"):
        k = self.k
        xT = k.sb([nch, T], BF16)
        ng = (nch + 7) // 8
        rs = XRes([Res("%s%d" % (name, g)) for g in range(ng)])

        def issue(g):
            c1 = min(nch, (g + 1) * 8)
            k.dma("sp", xT[:, g * 8:c1, :], rows_p(src[g * 1024:c1 * 128, :]), writes=[rs.parts[g]])
        issue(0)
        rs.rest = lambda: [issue(g) for g in range(1, ng)]
        return xT, rs

    def _wtile(self, ring, wh, row0, nk, col0, ncols):
        k = self.k
        buf, r = ring.next()
        v = buf[:, 0:nk, 0:ncols]
        blocks = wh["blocks"]
        r_w = wh["res"]
        if wh["kind"] == "row":
            rl = wh["rows_loc"]
            first = True
            for rk in range(row0 // rl, (row0 + nk * 128 - 1) // rl + 1):
                lo = max(row0, rk * rl)
                hi = min(row0 + nk * 128, (rk + 1) * rl)
                c_lo = (lo - row0) // 128
                n = (hi - lo) // 128
                k.dma("sp", buf[:, c_lo:c_lo + n, 0:ncols], rows_p(blocks[rk][lo - rk * rl:hi - rk * rl, col0:col0 + ncols]),
                      reads=[r_w], writes=[r], join=not first)
                first = False
        elif wh["kind"] == "col":
            cl = 512
            rk = col0 // cl
            c = col0 % cl
            k.dma("sp", v, rows_p(blocks[rk][row0:row0 + nk * 128, c:c + ncols]), reads=[r_w], writes=[r])
        else:
            e_ = row0 // D
            rr = row0 % D
            k.dma("sp", v, rows_p(blocks[e_][rr:rr + nk * 128, col0:col0 + ncols]), reads=[r_w], writes=[r])
        return v, r

    def ph_inproj(self, w_in, xnT, aT, pT, fT, uT, edges):
        k = self.k
        wf = w_in
        xT, r_x = self._load_xT(xnT)
        wring = Ring(k, 4, [32, 256], BF16, "w")
        sig_ring = Ring(k, 2, [512], F32, "sig")
        st_ring = Ring(k, 3, [2, T], F32, "stg")
        bank_ctr = [0]

        def nb():
            b = bank_ctr[0]
            bank_ctr[0] = (b + 1) % 8
            return b

        jobs = []
        for i in range(4):
            jobs.append(("glu", i))
        for kind, base in (("p", 2048), ("f", 3072), ("u", 4096)):
            for i in range(4):
                jobs.append((kind, base + i * 256, i))
        tiles = {}

        def load(j):
            job = jobs[j]
            if job[0] == "glu":
                i = job[1]
                t1 = self._wtile(wring, wf, 0, 32, i * 256, 256)
                t2 = self._wtile(wring, wf, 0, 32, 1024 + i * 256, 256)
                tiles[j] = (t1, t2)
            else:
                tiles[j] = (self._wtile(wring, wf, 0, 32, job[1], 256),)

        def comp(j):
            job = jobs[j]
            stg, r_stg = st_ring.next()
            if job[0] == "glu":
                i = job[1]
                (wl, r_wl), (wg, r_wg) = tiles[j]
                for oc in range(2):
                    for tt in range(2):
                        bl, bg = nb(), nb()
                        k.mm_group(k.ps(bl), bl, 32, lambda kc, oc=oc: wl[:, kc, oc * 128:(oc + 1) * 128],
                                   lambda kc, tt=tt: xT[:, kc, tt * 512:(tt + 1) * 512], lambda kc: [r_wl, r_x.kc(kc)])
                        k.mm_group(k.ps(bg), bg, 32, lambda kc, oc=oc: wg[:, kc, oc * 128:(oc + 1) * 128],
                                   lambda kc, tt=tt: xT[:, kc, tt * 512:(tt + 1) * 512], lambda kc: [r_wg, r_x.kc(kc)])
                        sg, r_sg = sig_ring.next()
                        k.op("act", lambda e, sg=sg, bg=bg: e.activation(out=sg, in_=k.ps(bg), func=AF.Sigmoid), reads=[k.psres[bg]], writes=[r_sg])
                        k.op("dve", lambda e, sg=sg, bl=bl, oc=oc, tt=tt: e.tensor_tensor(out=stg[:, oc, tt * 512:(tt + 1) * 512], in0=sg, in1=k.ps(bl), op=ALU.mult),
                             reads=[r_sg, k.psres[bl]], writes=[r_stg])
                rows = slice(i * 256, (i + 1) * 256)
                k.dma("sp", rows_p(aT[rows, :]), stg, reads=[r_stg])
                k.dma("sp", rows_p(edges[rows, 0:15]), stg[:, :, 0:15], reads=[r_stg])
                k.dma("sp", rows_p(edges[rows, 16:31]), stg[:, :, T - 15:T], reads=[r_stg])
            else:
                kind, col0, i = job
                (wt, r_wt), = tiles[j]
                if kind == "f":
                    stgb = stg.bitcast(BF16)[:, :, 0:T] if False else None
                for oc in range(2):
                    for tt in range(2):
                        b = nb()
                        k.mm_group(k.ps(b), b, 32, lambda kc, oc=oc: wt[:, kc, oc * 128:(oc + 1) * 128],
                                   lambda kc, tt=tt: xT[:, kc, tt * 512:(tt + 1) * 512], lambda kc, r_wt=r_wt: [r_wt, r_x.kc(kc)])
                        if kind == "f":
                            dst = self._bfview(stg)[:, oc, tt * 512:(tt + 1) * 512]
                        else:
                            dst = stg[:, oc, tt * 512:(tt + 1) * 512]
                        if (oc + tt) % 2 == 0:
                            k.op("act", lambda e, dst=dst, b=b: e.activation(out=dst, in_=k.ps(b), func=AF.Copy), reads=[k.psres[b]], writes=[r_stg])
                        else:
                            k.op("dve", lambda e, dst=dst, b=b: e.tensor_copy(out=dst, in_=k.ps(b)), reads=[k.psres[b]], writes=[r_stg])
                rows = slice(i * 256, (i + 1) * 256)
                if kind == "f":
                    k.dma("sp", rows_p(fT[rows, :]), self._bfview(stg)[:, :, 0:T], reads=[r_stg])
                elif kind == "u":
                    k.dma("sp", rows_p(uT[rows, :]), stg, reads=[r_stg])
                else:
                    k.dma("sp", rows_p(pT[rows, :]), stg, reads=[r_stg])
                    k.dma("sp", rows_p(edges[rows, 32:40]), stg[:, :, 0:8], reads=[r_stg])
                    k.dma("sp", rows_p(edges[rows, 40:48]), stg[:, :, T - 8:T], reads=[r_stg])

        pipeline([lambda j=j: load(j) for j in range(len(jobs))], [lambda j=j: comp(j) for j in range(len(jobs))], depth=1, after_first=r_x.rest)
        k.barrier()

    @staticmethod
    def _bfview(stg):
        return stg.bitcast(BF16)

    def ph_vproj(self, w_in, xnT, vn_dst, ln_g, ln_b):
        k = self.k
        wf = w_in
        xT, r_x = self._load_xT(xnT)
        wring = Ring(k, 3, [32, 256], BF16, "w")
        v = k.sb([8, BW], F32)
        r_v = [Res("v%d" % i) for i in range(4)]
        gb = k.sb([BW], F32); bb = k.sb([BW], F32); r_gb = Res("gb")
        k.dma("sp", gb, ln_g.partition_broadcast(128), writes=[r_gb])
        r_bb = Res("bb")
        k.dma("sp", bb, ln_b.partition_broadcast(128), writes=[r_bb])
        tiles = {}
        bank_ctr = [0]

        def load(ct):
            tiles[ct] = self._wtile(wring, wf, 0, 32, 5120 + ct * 256, 256)

        def comp(ct):
            wt, r_wt = tiles[ct]
            for tt in range(8):
                b = bank_ctr[0]
                bank_ctr[0] = (b + 1) % 8
                out = k.ps(b)[:, 0:256]
                k.mm_group(out, b, 32, lambda kc, tt=tt: xT[:, kc, tt * 128:(tt + 1) * 128], lambda kc: wt[:, kc, :], lambda kc, r_wt=r_wt: [r_wt, r_x.kc(kc)])
                dst = v[:, tt, ct * 256:(ct + 1) * 256]
                if tt % 2 == 0:
                    k.op("act", lambda e, dst=dst, out=out: e.activation(out=dst, in_=out, func=AF.Copy), reads=[k.psres[b]], writes=[r_v[ct]])
                else:
                    k.op("dve", lambda e, dst=dst, out=out: e.tensor_copy(out=dst, in_=out), reads=[k.psres[b]], writes=[r_v[ct]])

        pipeline([lambda c=c: load(c) for c in range(4)], [lambda c=c: comp(c) for c in range(4)], depth=2, after_first=r_x.rest)
        s1 = k.sb([8], F32); s2 = k.sb([8], F32); mean = k.sb([8], F32); msq = k.sb([8], F32)
        var = k.sb([8], F32); sd = k.sb([8], F32); rstd = k.sb([8], F32)
        sq = k.sb([8, BW], F32)
        r_s = Res("stats"); r_sq = Res("sq")
        k.op("dve", lambda e: e.reduce_sum(out=s1, in_=v, axis=AX.X), reads=r_v, writes=[r_s])
        k.op("dve", lambda e: e.tensor_tensor(out=sq, in0=v, in1=v, op=ALU.mult), reads=r_v, writes=[r_sq])
        k.op("dve", lambda e: e.reduce_sum(out=s2, in_=sq, axis=AX.X), reads=[r_sq], writes=[r_s])
        k.op("dve", lambda e: e.tensor_scalar(out=mean, in0=s1, scalar1=1.0 / BW, scalar2=None, op0=ALU.mult), reads=[r_s], writes=[r_s])
        k.op("dve", lambda e: e.tensor_tensor(out=msq, in0=mean, in1=mean, op=ALU.mult), reads=[r_s], writes=[r_s])
        k.op("dve", lambda e: e.scalar_tensor_tensor(out=var, in0=s2, scalar=1.0 / BW, in1=msq, op0=ALU.mult, op1=ALU.subtract), reads=[r_s], writes=[r_s])
        k.op("act", lambda e: e.activation(out=sd, in_=var, func=AF.Sqrt, bias=self.epsb[:, 0:1], scale=1.0), reads=[r_s], writes=[r_s])
        k.op("dve", lambda e: e.reciprocal(out=rstd, in_=sd), reads=[r_s], writes=[r_s])
        for tt in range(8):
            k.op("dve", lambda e, tt=tt: e.tensor_scalar(out=sq[:, tt, :], in0=v[:, tt, :], scalar1=mean[:, tt:tt + 1], scalar2=rstd[:, tt:tt + 1],
                                                          op0=ALU.subtract, op1=ALU.mult), reads=r_v + [r_s], writes=[r_sq])
        k.op("dve", lambda e: e.tensor_tensor(out=sq, in0=sq, in1=gb.unsqueeze(1).to_broadcast([128, 8, BW]), op=ALU.mult), reads=[r_sq, r_gb], writes=[r_sq])
        vb = k.sb([8, BW], BF16); r_vb = Res("vb")
        k.op("dve", lambda e: e.tensor_tensor(out=vb, in0=sq, in1=bb.unsqueeze(1).to_broadcast([128, 8, BW]), op=ALU.add), reads=[r_sq, r_bb], writes=[r_vb])
        k.dma("sp", vn_dst.rearrange("(t p) c -> p t c", p=128), vb, reads=[r_vb])
        k.barrier()

    def ph_four1(self, fT, cdft, xcs):
        k = self.k
        f, r_f = self._load_xT(fT, 8, "fT")
        r_f.rest()
        cd = k.sb([2, 512], BF16); r_cd = Res("cd")
        k.dma("sp", cd, rows_p(cdft), writes=[r_cd])
        oring = Ring(k, 2, [4, 512], BF16, "xo")
        bctr = 0
        for n in range(8):
            xo, r_xo = oring.next()
            for g in range(4):
                b = bctr
                bctr = (bctr + 1) % 8
                k.mm_group(k.ps(b), b, 2, lambda cc, g=g, n=n: f[:, 2 * g + cc, n * 128:(n + 1) * 128], lambda cc: cd[:, cc, :], [r_f.parts[0], r_cd])
                if g % 2 == 0:
                    k.op("act", lambda e, xo=xo, g=g, b=b: e.activation(out=xo[:, g, :], in_=k.ps(b), func=AF.Copy), reads=[k.psres[b]], writes=[r_xo])
                else:
                    k.op("dve", lambda e, xo=xo, g=g, b=b: e.tensor_copy(out=xo[:, g, :], in_=k.ps(b)), reads=[k.psres[b]], writes=[r_xo])
            k.dma("sp", xcs[n * 128:(n + 1) * 128, :].rearrange("p (g c) -> p g c", g=4), xo, reads=[r_xo])
        k.barrier()

    def _halo_load(self, dst, src_rows, cols, r_dst):
        k = self.k
        if src_rows is None:
            k.op("dve", lambda e: e.memset(dst, 0.0), writes=[r_dst])
        else:
            k.dma("sp", dst, rows_p(src_rows[:, cols]), writes=[r_dst])

    def ph_conv(self, aT, eL, eR, conv_wT, chanp, brT):
        k = self.k
        HW = 15
        aH = k.sb([8, T + 2 * HW], F32); r_a = Res("aH"); r_aL = Res("aHL"); r_aR = Res("aHR")
        k.dma("sp", aH[:, :, HW:HW + T], rows_p(aT), writes=[r_a])
        self._halo_load(aH[:, :, 0:HW], eL, slice(16, 31), r_aL)
        self._halo_load(aH[:, :, HW + T:HW + T + HW], eR, slice(0, 15), r_aR)
        cw = k.sb([8, 31], F32); r_cw = Res("cw")
        k.dma("sp", cw, rows_p(conv_wT), writes=[r_cw])
        cp = k.sb([32], F32); r_cp = Res("cp")
        k.dma("sp", cp, chanp, writes=[r_cp])
        acc = k.sb([8, T], F32)
        r_acc = [Res("acc%d" % c) for c in range(8)]
        for c in range(8):
            for j in range(31):
                src = aH[:, c, j:j + T]
                if j == 0:
                    k.op("dve", lambda e, c=c, src=src: e.tensor_scalar(out=acc[:, c, :], in0=src, scalar1=cw[:, c, 0:1], scalar2=cp[:, c:c + 1], op0=ALU.mult, op1=ALU.add),
                         reads=[r_a, r_aL, r_aR, r_cw, r_cp], writes=[r_acc[c]])
                else:
                    k.op("dve", lambda e, c=c, j=j, src=src: e.scalar_tensor_tensor(out=acc[:, c, :], in0=src, scalar=cw[:, c, j:j + 1], in1=acc[:, c, :], op0=ALU.mult, op1=ALU.add),
                         reads=[r_a, r_aL, r_aR, r_cw], writes=[r_acc[c]])
        sq = k.sb([8, T], F32); r_sq = [Res("sq%d" % c) for c in range(8)]
        for c in range(8):
            k.op("act", lambda e, c=c: e.activation(out=sq[:, c, :], in_=acc[:, c, :], func=AF.Square), reads=[r_acc[c]], writes=[r_sq[c]])
        mean = k.sb([T], F32); msq = k.sb([T], F32); var = k.sb([T], F32); sd = k.sb([T], F32); rstd = k.sb([T], F32)
        r_st = Res("st")
        for tt in range(2):
            b1, b2 = tt * 2, tt * 2 + 1
            k.mm_group(k.ps(b1), b1, 8, lambda c: self.onesf, lambda c, tt=tt: acc[:, c, tt * 512:(tt + 1) * 512], r_acc)
            k.mm_group(k.ps(b2), b2, 8, lambda c: self.onesf, lambda c, tt=tt: sq[:, c, tt * 512:(tt + 1) * 512], r_sq)
            sl = slice(tt * 512, (tt + 1) * 512)
            k.op("act", lambda e, sl=sl, b1=b1: e.activation(out=mean[:, sl], in_=k.ps(b1), func=AF.Copy, scale=1.0 / BW), reads=[k.psres[b1]], writes=[r_st])
            k.op("dve", lambda e, sl=sl: e.tensor_tensor(out=msq[:, sl], in0=mean[:, sl], in1=mean[:, sl], op=ALU.mult), reads=[r_st], writes=[r_st])
            k.op("dve", lambda e, sl=sl, b2=b2: e.scalar_tensor_tensor(out=var[:, sl], in0=k.ps(b2), scalar=1.0 / BW, in1=msq[:, sl], op0=ALU.mult, op1=ALU.subtract),
                 reads=[k.psres[b2], r_st], writes=[r_st])
            k.op("act", lambda e, sl=sl: e.activation(out=sd[:, sl], in_=var[:, sl], func=AF.Sqrt, bias=self.epsb[:, 0:1], scale=1.0), reads=[r_st], writes=[r_st])
            k.op("dve", lambda e, sl=sl: e.reciprocal(out=rstd[:, sl], in_=sd[:, sl]), reads=[r_st], writes=[r_st])
        oring = Ring(k, 2, [T], BF16, "ya")
        for c in range(8):
            k.op("dve", lambda e, c=c: e.tensor_tensor(out=sq[:, c, :], in0=acc[:, c, :], in1=mean, op=ALU.subtract), reads=[r_acc[c], r_st, r_sq[c]], writes=[r_sq[c]])
            k.op("dve", lambda e, c=c: e.tensor_tensor(out=sq[:, c, :], in0=sq[:, c, :], in1=rstd, op=ALU.mult), reads=[r_st], writes=[r_sq[c]])
            ya, r_ya = oring.next()
            k.op("act", lambda e, c=c, ya=ya: e.activation(out=ya, in_=sq[:, c, :], func=AF.Silu, scale=cp[:, 8 + c:9 + c], bias=cp[:, 16 + c:17 + c]),
                 reads=[r_sq[c], r_cp], writes=[r_ya])
            k.dma("sp", brT[c * 128:(c + 1) * 128, :], ya, reads=[r_ya])
        k.barrier()

    def ph_pool(self, pT, eL, eR, invcnt, pool_w, chanp, brT):
        k = self.k
        HW = 8
        pH = k.sb([8, T + 2 * HW], F32); r_p = Res("pH"); r_pL = Res("pHL"); r_pR = Res("pHR")
        k.dma("sp", pH[:, :, HW:HW + T], rows_p(pT), writes=[r_p])
        self._halo_load(pH[:, :, 0:HW], eL, slice(40, 48), r_pL)
        self._halo_load(pH[:, :, HW + T:HW + T + HW], eR, slice(32, 40), r_pR)
        r_pj = Res("pHj")
        k.op("dve", lambda e: e.tensor_copy(out=pH[:, 0, 0:1], in_=pH[:, 0, 0:1]), reads=[r_pL, r_pR, r_p], writes=[r_p])
        icbf = k.sb([4 * T], F32); r_ic = Res("icb")
        k.dma("sp", icbf, invcnt.partition_broadcast(128), writes=[r_ic])
        icb = icbf.rearrange("p (g t) -> p g t", g=4)
        cp = k.sb([32], F32); r_cp = Res("cp")
        k.dma("sp", cp, chanp, writes=[r_cp])
        pwf = k.sb([8, 256], F32); r_pwf = Res("pwf")
        k.dma("sp", pwf, rows_p(pool_w), writes=[r_pwf])
        pw = k.sb([8, 256], BF16); r_pw = Res("pw")
        k.op("act", lambda e: e.activation(out=pw, in_=pwf, func=AF.Copy), reads=[r_pwf], writes=[r_pw])
        A = k.sb([2, T + 16], F32); B = k.sb([2, T + 16], F32); r_A = Res("A"); r_B = Res("B")
        pooled_ring = Ring(k, 2, [2, T], BF16, "pooled")
        oring = Ring(k, 2, [T], BF16, "yb")
        bctr = 0
        for g, w in enumerate((2, 4, 8, 16)):
            X = pH[:, 2 * g:2 * g + 2, :]
            L0 = T + 16
            cur, r_cur, ln = X, r_p, L0
            step = 1
            bufs = [(A, r_A), (B, r_B)]
            bi = 0
            while step < w:
                dst, r_dst = bufs[bi]
                bi ^= 1
                nl = ln - step
                k.op("dve", lambda e, dst=dst, cur=cur, nl=nl, step=step: e.tensor_tensor(out=dst[:, :, 0:nl], in0=cur[:, :, 0:nl], in1=cur[:, :, step:step + nl], op=ALU.add),
                     reads=[r_cur], writes=[r_dst])
                cur, r_cur, ln = dst, r_dst, nl
                step *= 2
            off = HW - w // 2
            dst, r_dst = bufs[bi]
            k.op("dve", lambda e, dst=dst, cur=cur, off=off, g=g: e.tensor_tensor(out=dst[:, :, 0:T], in0=cur[:, :, off:off + T],
                                                                                 in1=icb[:, g, :].unsqueeze(1).to_broadcast([128, 2, T]), op=ALU.mult),
                 reads=[r_cur, r_ic], writes=[r_dst])
            pooled, r_pl = pooled_ring.next()
            k.op("dve", lambda e, dst=dst, X=X, pooled=pooled: e.tensor_tensor(out=pooled, in0=dst[:, :, 0:T], in1=X[:, :, HW:HW + T], op=ALU.subtract),
                 reads=[r_dst, r_p], writes=[r_pl])
            for ec in range(2):
                yb, r_yb = oring.next()
                ch = 2 * g + ec
                for tt in range(2):
                    b = bctr
                    bctr = (bctr + 1) % 8
                    k.mm_group(k.ps(b), b, 2, lambda cc, g=g, ec=ec: pw[:, 2 * g + cc, ec * 128:(ec + 1) * 128],
                               lambda cc, tt=tt, pooled=pooled: pooled[:, cc, tt * 512:(tt + 1) * 512], [r_pw, r_pl])
                    k.op("act", lambda e, yb=yb, tt=tt, b=b, ch=ch: e.activation(out=yb[:, tt * 512:(tt + 1) * 512], in_=k.ps(b), func=AF.Copy, scale=cp[:, 24 + ch:25 + ch]),
                         reads=[k.psres[b], r_cp], writes=[r_yb])
                k.dma("sp", brT[BW + ch * 128:BW + (ch + 1) * 128, :], yb, reads=[r_yb])
        k.barrier()

    def ph_gmlp(self, vn, uT, wsT, gb_vec, brT):
        k = self.k
        v = k.sb([8, BW], BF16); r_v = Res("vn")
        k.dma("sp", v, vn.rearrange("(n q) c -> q n c", q=128), writes=[r_v])
        u = k.sb([8, T], F32); r_u = Res("u")
        k.dma("sp", u, rows_p(uT), writes=[r_u])
        wsf = k.sb([8 * 128], F32); r_wsf = Res("wsf")
        k.dma("sp", wsf, wsT, writes=[r_wsf])
        ws = k.sb([8, 128], BF16); r_ws = Res("ws")
        k.op("act", lambda e: e.activation(out=ws, in_=wsf.rearrange("q (h p) -> q h p", h=8), func=AF.Copy), reads=[r_wsf], writes=[r_ws])
        bb = k.sb([8, 128], F32); r_bb = Res("bb")
        k.dma("sp", bb.rearrange("p h q -> p (h q)"), gb_vec.partition_broadcast(128), writes=[r_bb])
        tring = Ring(k, 2, [512], F32, "tmp")
        oring = Ring(k, 2, [T], BF16, "yd")
        for h in range(8):
            yd, r_yd = oring.next()
            for j in range(2):
                b = (h % 4) * 2 + j
                for n4 in range(4):
                    n = j * 4 + n4
                    k.op("pe", lambda e, b=b, n4=n4, n=n, h=h: e.matmul(k.ps(b)[:, n4 * 128:(n4 + 1) * 128], v[:, n, h * 128:(h + 1) * 128], ws[:, h, :], start=True, stop=True),
                         reads=[r_v, r_ws], writes=[k.psres[b]], signal=(n4 == 3))
                tmp, r_tmp = tring.next()
                k.op("dve", lambda e, b=b, h=h, tmp=tmp: e.tensor_tensor(out=tmp.rearrange("p (n q) -> p n q", q=128), in0=k.ps(b).rearrange("p (n q) -> p n q", q=128),
                                                                       in1=bb[:, h, :].unsqueeze(1).to_broadcast([128, 4, 128]), op=ALU.add),
                     reads=[k.psres[b], r_bb], writes=[r_tmp])
                k.op("dve", lambda e, tmp=tmp, yd=yd, h=h, j=j: e.tensor_tensor(out=yd[:, j * 512:(j + 1) * 512], in0=tmp, in1=u[:, h, j * 512:(j + 1) * 512], op=ALU.mult),
                     reads=[r_tmp, r_u], writes=[r_yd])
            k.dma("sp", brT[3 * BW + h * 128:3 * BW + (h + 1) * 128, :], yd, reads=[r_yd])
        k.barrier()

    def ph_four2(self, xcs_g, dftab, brT):
        k = self.k
        xring = Ring(k, 2, [32, 512], BF16, "X")
        tring = Ring(k, 3, [4, 2048], BF16, "tab")
        oring = Ring(k, 2, [T], BF16, "yc")
        r_tab = Res("dftab_in")
        for g in range(4):
            X, r_X = xring.next()
            k.dma("sp", X, rows_p(xcs_g[:, g * 512:(g + 1) * 512]), writes=[r_X])
            banks = [(g % 2) * 4 + i for i in range(4)]
            tabs = {}

            def load(sb):
                tb, r_tb = tring.next()
                tabs[sb] = (tb, r_tb)
                k.dma("sp", tb, rows_p(dftab[sb * 512:(sb + 1) * 512, :]), writes=[r_tb])

            def comp(sb, X=X, r_X=r_X, banks=banks):
                tb, r_tb = tabs[sb]
                for sc in range(4):
                    s = sb * 4 + sc
                    for cs in range(2):
                        for cc in range(2):
                            for kt in range(2):
                                b = banks[cc * 2 + kt]
                                first = (s == 0 and cs == 0)
                                last = (s == 31 and cs == 1)
                                k.op("pe", lambda e, b=b, s=s, cs=cs, cc=cc, kt=kt, sc=sc, tb=tb, first=first, last=last:
                                     e.matmul(k.ps(b), X[:, s, cs * 256 + cc * 128:cs * 256 + (cc + 1) * 128], tb[:, sc, cs * 1024 + kt * 512:cs * 1024 + (kt + 1) * 512], start=first, stop=last),
                                     reads=[r_X, r_tb], writes=[k.psres[b]], signal=(last or (sc == 3 and cs == 1 and cc == 1 and kt == 1)))
            pipeline([lambda sb=sb: load(sb) for sb in range(8)], [lambda sb=sb: comp(sb) for sb in range(8)], depth=2)
            for cc in range(2):
                yc, r_yc = oring.next()
                for kt in range(2):
                    b = banks[cc * 2 + kt]
                    k.op("act", lambda e, yc=yc, kt=kt, b=b: e.activation(out=yc[:, kt * 512:(kt + 1) * 512], in_=k.ps(b), func=AF.Copy, scale=1.0 / 1024.0),
                         reads=[k.psres[b]], writes=[r_yc])
                row = 2 * BW + g * 256 + cc * 128
                k.dma("sp", brT[row:row + 128, :], yc, reads=[r_yc])
        k.barrier()

    def ph_merge(self, w_gate, w_branch, xnT, brT, mergedT):
        k = self.k
        wg_f = w_gate
        wb_f = w_branch
        xT, r_x = self._load_xT(xnT)
        gring = Ring(k, 2, [32, 256], BF16, "wg")
        bring = Ring(k, 2, [8, 256], BF16, "wb")
        brring = Ring(k, 2, [8, T], BF16, "br")
        accring = Ring(k, 2, [2, T], F32, "acc")
        sring = Ring(k, 2, [512], F32, "sig")
        tring = Ring(k, 2, [512], F32, "tmp")
        mring = Ring(k, 2, [2, T], BF16, "mo")
        jobs = [(dp, g) for dp in range(16) for g in range(4)]
        tiles = {}
        bctr = [0]

        def nb():
            b = bctr[0]
            bctr[0] = (b + 1) % 8
            return b

        def load(j):
            dp, g = jobs[j]
            t1 = self._wtile(gring, wg_f, 0, 32, g * D + dp * 256, 256)
            t2 = self._wtile(bring, wb_f, g * BW, 8, dp * 256, 256)
            br, r_br = brring.next()
            k.dma("sp", br, rows_p(brT[g * BW:(g + 1) * BW, :]), writes=[r_br])
            tiles[j] = (t1, t2, (br, r_br))

        cur = {}

        def comp(j):
            dp, g = jobs[j]
            (wg, r_wgt), (wb, r_wbt), (br, r_br) = tiles[j]
            if g == 0:
                cur["acc"] = accring.next()
            acc, r_acc = cur["acc"]
            for oc in range(2):
                for tt in range(2):
                    bg, bp = nb(), nb()
                    k.mm_group(k.ps(bg), bg, 32, lambda kc, oc=oc: wg[:, kc, oc * 128:(oc + 1) * 128],
                               lambda kc, tt=tt: xT[:, kc, tt * 512:(tt + 1) * 512], lambda kc, r_wgt=r_wgt: [r_wgt, r_x.kc(kc)])
                    k.mm_group(k.ps(bp), bp, 8, lambda kc, oc=oc: wb[:, kc, oc * 128:(oc + 1) * 128],
                               lambda kc, tt=tt: br[:, kc, tt * 512:(tt + 1) * 512], [r_wbt, r_br])
                    sg, r_sg = sring.next()
                    k.op("act", lambda e, sg=sg, bg=bg: e.activation(out=sg, in_=k.ps(bg), func=AF.Sigmoid), reads=[k.psres[bg]], writes=[r_sg])
                    dst = acc[:, oc, tt * 512:(tt + 1) * 512]
                    if g == 0:
                        k.op("dve", lambda e, sg=sg, bp=bp, dst=dst: e.tensor_tensor(out=dst, in0=sg, in1=k.ps(bp), op=ALU.mult),
                             reads=[r_sg, k.psres[bp]], writes=[r_acc])
                    else:
                        tmp, r_tmp = tring.next()
                        k.op("dve", lambda e, sg=sg, bp=bp, tmp=tmp: e.tensor_tensor(out=tmp, in0=sg, in1=k.ps(bp), op=ALU.mult),
                             reads=[r_sg, k.psres[bp]], writes=[r_tmp])
                        k.op("dve", lambda e, tmp=tmp, dst=dst: e.tensor_tensor(out=dst, in0=dst, in1=tmp, op=ALU.add),
                             reads=[r_tmp], writes=[r_acc])
            if g == 3:
                mo, r_mo = mring.next()
                k.op("act", lambda e, mo=mo, acc=acc: e.activation(out=mo, in_=acc, func=AF.Copy), reads=[r_acc], writes=[r_mo])
                k.dma("sp", rows_p(mergedT[dp * 256:(dp + 1) * 256, :]), mo, reads=[r_mo])

        pipeline([lambda j=j: load(j) for j in range(len(jobs))], [lambda j=j: comp(j) for j in range(len(jobs))], depth=1, after_first=r_x.rest)
        k.barrier()

    def ph_outproj(self, w2, kc0, nk, actT_src, h_src, h_dst):
        k = self.k
        wf = w2
        aT_, r_a = self._load_xT(actT_src, nk, "actT")
        wring = Ring(k, 3, [32, 256], BF16, "w")
        hin_ring = Ring(k, 3, [8, 256], F32, "hin")
        hout_ring = Ring(k, 2, [8, 256], F32, "hout")
        tiles = {}
        bctr = [0]

        def load(ct):
            wt = self._wtile(wring, wf, kc0 * 128, nk, ct * 256, 256)
            hin, r_hin = hin_ring.next()
            k.dma("sp", hin, h_src[:, ct * 256:(ct + 1) * 256].rearrange("(t p) c -> p t c", p=128), writes=[r_hin])
            tiles[ct] = (wt, (hin, r_hin))

        def comp(ct):
            (wt, r_wt), (hin, r_hin) = tiles[ct]
            hout, r_hout = hout_ring.next()
            for tt in range(8):
                b = bctr[0]
                bctr[0] = (b + 1) % 8
                out = k.ps(b)[:, 0:256]
                k.mm_group(out, b, nk, lambda kc, tt=tt: aT_[:, kc, tt * 128:(tt + 1) * 128], lambda kc: wt[:, kc, :], lambda kc, r_wt=r_wt: [r_wt, r_a.kc(kc)])
                k.op("dve", lambda e, out=out, tt=tt, hin=hin, hout=hout: e.tensor_tensor(out=hout[:, tt, :], in0=out, in1=hin[:, tt, :], op=ALU.add),
                     reads=[k.psres[b], r_hin], writes=[r_hout])
            k.dma("sp", h_dst[:, ct * 256:(ct + 1) * 256].rearrange("(t p) c -> p t c", p=128), hout, reads=[r_hout])

        pipeline([lambda c=c: load(c) for c in range(16)], [lambda c=c: comp(c) for c in range(16)], depth=2, after_first=r_a.rest)
        k.barrier()

    def ph_ffn1(self, w1, w3, c0, nch, xnT, GT, cmb_row, wrow0=0):
        k = self.k
        w1f = w1
        w3f = w3
        xT, r_x = self._load_xT(xnT)
        wring = Ring(k, 4, [32, 256], BF16, "w")
        sring = Ring(k, 2, [512], F32, "sil")
        tring = Ring(k, 2, [512], F32, "tmp")
        gring = Ring(k, 2, [2, T], BF16, "g")
        cb = None
        if cmb_row is not None:
            cb = k.sb([T], F32); r_cb = Res("cb")
            k.dma("sp", cb, cmb_row.partition_broadcast(128), writes=[r_cb])
        tiles = {}
        bctr = [0]

        def nb():
            b = bctr[0]
            bctr[0] = (b + 1) % 8
            return b

        def load(j):
            col = (c0 + 2 * j) * 128
            tiles[j] = (self._wtile(wring, w1f, wrow0, 32, col, 256), self._wtile(wring, w3f, wrow0, 32, col, 256))

        def comp(j):
            (wa, r_wa), (wb, r_wbt) = tiles[j]
            gt, r_gt = gring.next()
            for oc in range(2):
                for tt in range(2):
                    ba, bb_ = nb(), nb()
                    k.mm_group(k.ps(ba), ba, 32, lambda kc, oc=oc: wa[:, kc, oc * 128:(oc + 1) * 128],
                               lambda kc, tt=tt: xT[:, kc, tt * 512:(tt + 1) * 512], lambda kc, r_wa=r_wa: [r_wa, r_x.kc(kc)])
                    k.mm_group(k.ps(bb_), bb_, 32, lambda kc, oc=oc: wb[:, kc, oc * 128:(oc + 1) * 128],
                               lambda kc, tt=tt: xT[:, kc, tt * 512:(tt + 1) * 512], lambda kc, r_wbt=r_wbt: [r_wbt, r_x.kc(kc)])
                    sl, r_sl = sring.next()
                    k.op("act", lambda e, sl=sl, ba=ba: e.activation(out=sl, in_=k.ps(ba), func=AF.Silu), reads=[k.psres[ba]], writes=[r_sl])
                    dst = gt[:, oc, tt * 512:(tt + 1) * 512]
                    if cb is None:
                        k.op("dve", lambda e, sl=sl, bb_=bb_, dst=dst: e.tensor_tensor(out=dst, in0=sl, in1=k.ps(bb_), op=ALU.mult),
                             reads=[r_sl, k.psres[bb_]], writes=[r_gt])
                    else:
                        tmp, r_tmp = tring.next()
                        k.op("dve", lambda e, sl=sl, bb_=bb_, tmp=tmp: e.tensor_tensor(out=tmp, in0=sl, in1=k.ps(bb_), op=ALU.mult),
                             reads=[r_sl, k.psres[bb_]], writes=[r_tmp])
                        k.op("dve", lambda e, tmp=tmp, dst=dst, tt=tt: e.tensor_tensor(out=dst, in0=tmp, in1=cb[:, tt * 512:(tt + 1) * 512], op=ALU.mult),
                             reads=[r_tmp, r_cb], writes=[r_gt])
            k.dma("sp", rows_p(GT[j * 256:(j + 1) * 256, :]), gt, reads=[r_gt])

        nj = nch // 2
        pipeline([lambda j=j: load(j) for j in range(nj)], [lambda j=j: comp(j) for j in range(nj)], depth=1, after_first=r_x.rest)
        k.barrier()

    def ph_router(self, h_src, gvec, routerT, cmbT):
        k = self.k
        gbc = k.sb([D], F32); r_g = Res("gbc")
        k.dma("sp", gbc, gvec.partition_broadcast(128), writes=[r_g])
        gr = k.sb([NE, D], F32); r_gr = [Res("gr%d" % e_) for e_ in range(NE)]
        for e_ in range(NE):
            k.dma("sp", gr[:, e_, :], routerT[e_ * D:(e_ + 1) * D].partition_broadcast(128), writes=[r_gr[e_]])
            k.op("dve", lambda e, e_=e_: e.tensor_tensor(out=gr[:, e_, :], in0=gr[:, e_, :], in1=gbc, op=ALU.mult), reads=[r_g], writes=[r_gr[e_]])
        hring = Ring(k, 2, [D], F32, "ht")
        prod = k.sb([D], F32); r_prod = Res("prod")
        junk, r_junk = prod, r_prod
        ss = k.sb([8], F32); sd = k.sb([8], F32); rstd = k.sb([8], F32)
        r_ss = Res("ss")
        lg = k.sb([8, NE], F32); r_lg = Res("lg")
        for tt in range(8):
            ht, r_ht = hring.next()
            k.dma("sp", ht, h_src[tt * 128:(tt + 1) * 128, :], writes=[r_ht])
            k.op("act", lambda e, ht=ht, tt=tt: e.activation(out=junk, in_=ht, func=AF.Square, accum_out=ss[:, tt:tt + 1]), reads=[r_ht], writes=[r_junk, r_ss])
            for e_ in range(NE):
                k.op("dve", lambda e, ht=ht, e_=e_: e.tensor_tensor(out=prod, in0=ht, in1=gr[:, e_, :], op=ALU.mult), reads=[r_ht, r_gr[e_]], writes=[r_prod])
                k.op("dve", lambda e, tt=tt, e_=e_: e.reduce_sum(out=lg[:, tt, e_:e_ + 1], in_=prod, axis=AX.X), reads=[r_prod], writes=[r_lg])
        k.op("act", lambda e: e.activation(out=sd, in_=ss, func=AF.Sqrt, bias=self.epsb[:, 0:1], scale=1.0 / D), reads=[r_ss], writes=[r_ss])
        k.op("dve", lambda e: e.reciprocal(out=rstd, in_=sd), reads=[r_ss], writes=[r_ss])
        r_t = Res("top")
        Lg = k.sb([8, NE], F32); m1 = k.sb([8], F32); m2 = k.sb([8], F32); eq1 = k.sb([8, NE], F32); eq2 = k.sb([8, NE], F32)
        L2 = k.sb([8, NE], F32); dd = k.sb([8], F32); w1 = k.sb([8], F32); w2 = k.sb([8], F32); cmb = k.sb([8, NE], F32); t2 = k.sb([8, NE], F32)

        def bc(a):
            return a.unsqueeze(2).to_broadcast([128, 8, NE])
        k.op("dve", lambda e: e.tensor_tensor(out=Lg, in0=lg, in1=bc(rstd), op=ALU.mult), reads=[r_lg, r_ss], writes=[r_t])
        k.op("dve", lambda e: e.reduce_max(out=m1, in_=Lg, axis=AX.X), reads=[r_t], writes=[r_t])
        k.op("dve", lambda e: e.tensor_tensor(out=eq1, in0=Lg, in1=bc(m1), op=ALU.is_equal), reads=[r_t], writes=[r_t])
        k.op("dve", lambda e: e.scalar_tensor_tensor(out=L2, in0=eq1, scalar=-1e30, in1=Lg, op0=ALU.mult, op1=ALU.add), reads=[r_t], writes=[r_t])
        k.op("dve", lambda e: e.reduce_max(out=m2, in_=L2, axis=AX.X), reads=[r_t], writes=[r_t])
        k.op("dve", lambda e: e.tensor_tensor(out=eq2, in0=L2, in1=bc(m2), op=ALU.is_equal), reads=[r_t], writes=[r_t])
        k.op("dve", lambda e: e.tensor_tensor(out=dd, in0=m1, in1=m2, op=ALU.subtract), reads=[r_t], writes=[r_t])
        k.op("act", lambda e: e.activation(out=w1, in_=dd, func=AF.Sigmoid), reads=[r_t], writes=[r_t])
        k.op("dve", lambda e: e.tensor_scalar(out=w2, in0=w1, scalar1=-1.0, scalar2=1.0, op0=ALU.mult, op1=ALU.add), reads=[r_t], writes=[r_t])
        k.op("dve", lambda e: e.tensor_tensor(out=cmb, in0=eq1, in1=bc(w1), op=ALU.mult), reads=[r_t], writes=[r_t])
        k.op("dve", lambda e: e.tensor_tensor(out=t2, in0=eq2, in1=bc(w2), op=ALU.mult), reads=[r_t], writes=[r_t])
        k.op("dve", lambda e: e.tensor_tensor(out=cmb, in0=cmb, in1=t2, op=ALU.add), reads=[r_t], writes=[r_t])
        cT = k.sb([T], F32); r_cT = Res("cT")
        for tt in range(8):
            b = tt // 4
            k.op("pe", lambda e, tt=tt, b=b: e.transpose(k.ps(b)[0:NE, (tt % 4) * 128:(tt % 4 + 1) * 128], cmb[:, tt, :], self.identf),
                 reads=[r_t], writes=[k.psres[b]], signal=(tt % 4 == 3))
        for b in range(2):
            k.op("act", lambda e, b=b: e.activation(out=cT[0:NE, b * 512:(b + 1) * 512], in_=k.ps(b)[0:NE, :], func=AF.Copy), reads=[k.psres[b]], writes=[r_cT])
        k.dma("sp", cmbT, cT[0:NE, :], reads=[r_cT])
        k.barrier()


_BF = ml_dtypes.bfloat16


_CONSTS = {}


def _host_consts():
    if "c" in _CONSTS:
        return _CONSTS["c"]
    tabs = []
    invs = []
    s_ = np.arange(S, dtype=np.int64)[:, None]
    for q in range(NBLK):
        kk = (q * T + np.arange(T, dtype=np.int64))[None, :]
        ang = 2.0 * np.pi * ((s_ * kk) % S).astype(np.float64) / S
        tabs.append(np.concatenate([np.cos(ang), -np.sin(ang)], axis=1).astype(np.float32).astype(_BF))
        pos = q * T + np.arange(T)
        for w in (2, 4, 8, 16):
            lo = np.clip(pos - w // 2, 0, S - 1)
            hi = np.clip(pos + w // 2 - 1, 0, S - 1)
            invs.append(1.0 / (hi - lo + 1).astype(np.float32))
    dftab = np.concatenate(tabs, axis=0)
    cc = np.arange(256, dtype=np.int64)
    ang2 = 2.0 * np.pi * ((cc[:, None] * cc[None, :]) % 256).astype(np.float64) / 256
    cdft = np.concatenate([np.cos(ang2), np.sin(ang2)], axis=1).astype(np.float32).astype(_BF)
    invcnt = np.concatenate(invs).astype(np.float32)
    _CONSTS["c"] = {"ident": np.eye(128, dtype=np.float32), "dftab": dftab, "cdft": cdft, "invcnt": invcnt}
    return _CONSTS["c"]


def _pc(v):
    return np.ascontiguousarray(np.asarray(v, np.float32).reshape(8, 128).T)


def make_in_maps(inputs, names):
    x = np.asarray(inputs["x"], np.float32).reshape(NCORES, NBLK * T, D)
    shared = dict(_host_consts())
    if "final_norm" in names:
        shared["final_norm"] = np.asarray(inputs["final_norm"], np.float32)
    for li in range(2):
        p = "l%d_" % li
        if p + "norm1" not in names:
            continue

        def g(nm):
            return np.asarray(inputs[p + nm], np.float32)
        shared[p + "norm1"] = g("norm1")
        shared[p + "norm2"] = g("norm2")
        shared[p + "conv_wT"] = np.ascontiguousarray(g("conv_w").T)
        shared[p + "chanp"] = np.ascontiguousarray(np.concatenate([_pc(g("conv_b")), _pc(g("conv_ln_g")), _pc(g("conv_ln_b")), _pc(g("pool_scale"))], axis=1))
        shared[p + "pool_w"] = g("pool_w").reshape(4 * 256, 256)
        shared[p + "gmlp_ln_g"] = g("gmlp_ln_g")
        shared[p + "gmlp_ln_b"] = g("gmlp_ln_b")
        shared[p + "gmlp_wsT"] = np.ascontiguousarray(g("gmlp_ws").transpose(2, 0, 1).reshape(128, 8 * 128))
        shared[p + "gmlp_b"] = g("gmlp_b").reshape(8 * 128)
        for nm in ("w_in", "w_gate", "w_out", "ffn_w1", "ffn_w3", "ffn_w2"):
            if p + nm in names:
                shared[p + nm] = g(nm)
        if p + "w_branch" in names:
            shared[p + "w_branch"] = g("w_branch").reshape(4 * BW, D)
        if p + "routerT" in names:
            shared[p + "routerT"] = np.ascontiguousarray(g("router").T).reshape(NE * D)
        for nm in ("exp_w1", "exp_w3", "exp_w2"):
            if p + nm in names:
                shared[p + nm] = g(nm).reshape(NE * D, D)
    maps = []
    for c in range(NCORES):
        m = dict(shared)
        m["x"] = x[c]
        maps.append(m)
    return maps


_ALL_INPUTS = (
    "x",
    "l0_norm1",
    "l0_w_in",
    "l0_w_gate",
    "l0_conv_w",
    "l0_conv_b",
    "l0_conv_ln_g",
    "l0_conv_ln_b",
    "l0_pool_w",
    "l0_pool_scale",
    "l0_gmlp_ln_g",
    "l0_gmlp_ln_b",
    "l0_gmlp_ws",
    "l0_gmlp_b",
    "l0_w_branch",
    "l0_w_out",
    "l0_norm2",
    "l0_ffn_w1",
    "l0_ffn_w3",
    "l0_ffn_w2",
    "l1_norm1",
    "l1_w_in",
    "l1_w_gate",
    "l1_conv_w",
    "l1_conv_b",
    "l1_conv_ln_g",
    "l1_conv_ln_b",
    "l1_pool_w",
    "l1_pool_scale",
    "l1_gmlp_ln_g",
    "l1_gmlp_ln_b",
    "l1_gmlp_ws",
    "l1_gmlp_b",
    "l1_w_branch",
    "l1_w_out",
    "l1_norm2",
    "l1_router",
    "l1_exp_w1",
    "l1_exp_w3",
    "l1_exp_w2",
    "final_norm",
)


_CACHE = {}


def kernel(**inputs):
    for nm in _ALL_INPUTS:
        assert nm in inputs, "missing input " + nm
    if "prog" not in _CACHE:
        pr = Prog()
        pr.build()
        _CACHE["prog"] = pr
    pr = _CACHE["prog"]
    maps = make_in_maps(inputs, set(pr.inp.keys()))
    res = run_bass_kernel_spmd(pr.nc, maps, core_ids=list(range(NCORES)))
    out = np.stack([np.asarray(r["out"], np.float32) for r in res.results], axis=0)
    return out.reshape(2, S, D)
```

```python
import numpy as np
from contextlib import ExitStack
import ml_dtypes
import concourse.bass as bass
import concourse.mybir as mybir
from concourse.bass_utils import run_bass_kernel_spmd

F32 = mybir.dt.float32
BF16 = mybir.dt.bfloat16
U8 = mybir.dt.uint8
AF = mybir.ActivationFunctionType
ALU = mybir.AluOpType
AX = mybir.AxisListType

NCORES = 2
NBLK = 4
D = 4096
T = 1024
S = 4096
BW = 1024
DFF = 11008
NE = 8
EPS = 1e-6
ND = 8
SB_BYTES = 206 * 1024


class Res:
    __slots__ = ("name", "w", "r", "epoch", "persist", "pend")

    def __init__(self, name="", persist=False):
        self.name = name
        self.w = []
        self.r = []
        self.epoch = 0
        self.persist = persist
        self.pend = None


class KB:
    def __init__(self, nc, st):
        self.nc = nc
        self.engs = {"pe": nc.tensor, "act": nc.scalar, "dve": nc.vector, "pool": nc.gpsimd, "sp": nc.sync}
        self.csem = {}
        self.ccnt = {}
        for e in ("pe", "act", "dve", "pool"):
            self.csem[e] = st.enter_context(nc.semaphore("c_" + e))
            self.ccnt[e] = 0
        self.dsem = {}
        self.dcnt = {}
        self.dnext = {}
        for q in ("sp", "act", "pool"):
            self.dsem[q] = [st.enter_context(nc.semaphore("d_%s%d" % (q, i))) for i in range(ND)]
            self.dcnt[q] = [0] * ND
            self.dnext[q] = 0
        self.ccsem = st.enter_context(nc.semaphore("ccs"))
        self.cccnt = 0
        self.known = {e: {} for e in self.engs}
        self.stream = {e: [] for e in self.engs}
        self.pending = {e: [] for e in self.engs}
        self.epoch = 1
        self.ninst = 0
        self.arena = st.enter_context(nc.sbuf_tensor("arena", [128, SB_BYTES], U8))
        self.sb_off = 0
        self.sb_base = 0
        self.psum = [st.enter_context(nc.psum_tensor("ps%d" % i, [128, 512], F32)) for i in range(8)]
        self.psres = [Res("ps%d" % i) for i in range(8)]

    def sb(self, shape, dtype, persist=False):
        esz = 4 if dtype == F32 else (2 if dtype == BF16 else 1)
        n = 1
        for s_ in shape:
            n *= s_
        nbytes = (n * esz + 63) // 64 * 64
        off = self.sb_off
        self.sb_off += nbytes
        assert self.sb_off <= SB_BYTES, "SBUF overflow %d" % self.sb_off
        ap = self.arena[:, off:off + n * esz]
        if dtype != U8:
            ap = ap.bitcast(dtype)
        if len(shape) == 2:
            ap = ap.rearrange("p (a b) -> p a b", b=shape[1])
        elif len(shape) == 3:
            ap = ap.rearrange("p (a b c) -> p a b c", b=shape[1], c=shape[2])
        if persist:
            self.sb_base = self.sb_off
        return ap

    def ps(self, i, dtype=F32):
        ap = self.psum[i][:, :]
        if dtype == BF16:
            ap = ap.bitcast(BF16)
        return ap

    def _touch(self, r):
        if r.epoch != self.epoch and not r.persist:
            r.w = []
            r.r = []
            r.pend = None
        r.epoch = self.epoch

    def _deps(self, e, reads, writes, join=False):
        evs = []
        for r in reads:
            self._touch(r)
            assert r.pend is None or r.pend == e, "pending unsignaled access on %s by %s" % (r.name, r.pend)
            evs.extend(r.w)
        for w in writes:
            self._touch(w)
            assert w.pend is None or w.pend == e, "pending unsignaled access on %s by %s" % (w.name, w.pend)
            if not join:
                evs.extend(w.w)
            evs.extend(w.r)
        kn = self.known[e]
        best = {}
        for (sem, val, owner) in evs:
            if owner == e and e == "pe":
                continue
            k = id(sem)
            if kn.get(k, 0) >= val:
                continue
            if k not in best or best[k][1] < val:
                best[k] = (sem, val)
        out = []
        for k, (sem, val) in best.items():
            kn[k] = val
            out.append((sem, val))
        return out

    def _attach(self, ev, reads, writes, join=False):
        for r in reads:
            r.r = [x for x in r.r if x[0] is not ev[0]] + [ev]
            r.pend = None
        for w in writes:
            if join:
                w.w = w.w + [ev]
            else:
                w.w = [ev]
            w.r = []
            w.pend = None

    def op(self, e, fn, reads=(), writes=(), signal=True):
        reads = list(reads)
        writes = list(writes)
        waits = self._deps(e, reads, writes)
        sem = self.csem[e]
        if signal:
            self.ccnt[e] += 1
            ev = (sem, self.ccnt[e], e)
            pr = [x for x, k in self.pending[e] if k == "r"]
            pw = [x for x, k in self.pending[e] if k == "w"]
            self.pending[e] = []
            self._attach(ev, pr + reads, pw + writes)
        else:
            for r in reads:
                r.pend = e
                self.pending[e].append((r, "r"))
            for w in writes:
                w.pend = e
                self.pending[e].append((w, "w"))
        self.ninst += 1

        def thunk(eng):
            for s_, v in waits:
                eng.wait_ge(s_, v)
            ins = fn(eng)
            if signal:
                ins.then_inc(sem, 1)
        self.stream[e].append(thunk)

    def dma(self, q, out, in_, reads=(), writes=(), join=False, **kw):
        reads = list(reads)
        writes = list(writes)
        i = self.dnext[q]
        self.dnext[q] = (i + 1) % ND
        sem = self.dsem[q][i]
        prev = self.dcnt[q][i]
        waits = self._deps(q, reads, writes, join=join)
        if prev > 0 and self.known[q].get(id(sem), 0) < prev:
            waits.append((sem, prev))
            self.known[q][id(sem)] = prev
        self.dcnt[q][i] = prev + 16
        ev = (sem, prev + 16, "dma")
        self._attach(ev, reads, writes, join=join)
        self.ninst += 1

        def thunk(eng):
            for s_, v in waits:
                eng.wait_ge(s_, v)
            eng.dma_start(out=out, in_=in_, **kw).then_inc(sem, 16)
        self.stream[q].append(thunk)

    def cc(self, kind, ins, outs, groups, reads=(), writes=()):
        reads = list(reads)
        writes = list(writes)
        waits = self._deps("pool", reads, writes)
        if self.cccnt > 0 and self.known["pool"].get(id(self.ccsem), 0) < self.cccnt:
            waits.append((self.ccsem, self.cccnt))
            self.known["pool"][id(self.ccsem)] = self.cccnt
        self.cccnt += 1
        sem = self.ccsem
        ev = (sem, self.cccnt, "cc")
        self._attach(ev, reads, writes)

        def thunk(eng):
            for s_, v in waits:
                eng.wait_ge(s_, v)
            eng.collective_compute(kind, ALU.bypass, replica_groups=groups, ins=ins, outs=outs).then_inc(sem, 1)
        self.stream["pool"].append(thunk)

    def barrier(self):
        for e in self.engs:
            assert not self.pending[e], "pending at barrier on " + e
        targets = []
        for e in ("pe", "act", "dve"):
            if self.ccnt[e] > 0:
                targets.append((self.csem[e], self.ccnt[e]))
        for q in ("sp", "act"):
            for i in range(ND):
                if self.dcnt[q][i] > 0:
                    targets.append((self.dsem[q][i], self.dcnt[q][i]))
        for e in ("pe", "act", "dve", "sp"):
            kn = self.known[e]
            waits = []
            for sem, v in targets:
                if kn.get(id(sem), 0) < v:
                    kn[id(sem)] = v
                    waits.append((sem, v))

            def thunk(eng, waits=waits):
                for s_, v in waits:
                    eng.wait_ge(s_, v)
            self.stream[e].append(thunk)
        self.epoch += 1
        self.sb_off = self.sb_base

    def finish(self):
        nc = self.nc
        fin = []
        if self.cccnt > 0:
            fin.append((self.ccsem, self.cccnt))
        for i in range(ND):
            if self.dcnt["pool"][i] > 0:
                fin.append((self.dsem["pool"][i], self.dcnt["pool"][i]))
        for e in ("pool", "sp"):
            def thunk(eng, fin=fin):
                for s_, v in fin:
                    eng.wait_ge(s_, v)
            self.stream[e].append(thunk)
        with nc.Block() as block:
            for e, deco in (("sp", block.sync), ("act", block.scalar), ("dve", block.vector),
                            ("pool", block.gpsimd), ("pe", block.tensor)):
                thunks = self.stream[e]
                self.stream[e] = []

                def run(eng, thunks=thunks):
                    for t_ in thunks:
                        t_(eng)
                deco(run)

    def pool_wait_all(self):
        for e in ("pe", "act", "dve", "sp"):
            assert not self.pending[e]
        targets = []
        for e in ("pe", "act", "dve"):
            if self.ccnt[e] > 0:
                targets.append((self.csem[e], self.ccnt[e]))
        for q in ("sp", "act"):
            for i in range(ND):
                if self.dcnt[q][i] > 0:
                    targets.append((self.dsem[q][i], self.dcnt[q][i]))
        kn = self.known["pool"]
        waits = []
        for sem, v in targets:
            if kn.get(id(sem), 0) < v:
                kn[id(sem)] = v
                waits.append((sem, v))

        def thunk(eng, waits=waits):
            for s_, v in waits:
                eng.wait_ge(s_, v)
        self.stream["pool"].append(thunk)

    def mm_group(self, out_ap, bank, n, lhs_fn, rhs_fn, reads):
        for i in range(n):
            rd = reads(i) if callable(reads) else reads
            self.op("pe", lambda e, i=i: e.matmul(out_ap, lhs_fn(i), rhs_fn(i), start=(i == 0), stop=(i == n - 1)),
                    reads=rd, writes=[self.psres[bank]], signal=(i == n - 1))


class XRes:
    def __init__(self, parts):
        self.parts = parts

    def kc(self, i):
        return self.parts[i // 8]


def pipeline(loads, computes, depth=2, after_first=None):
    n = len(computes)
    loads[0]()
    if after_first is not None:
        after_first()
    for j in range(1, min(depth, n)):
        loads[j]()
    for j in range(n):
        if j + depth < n:
            loads[j + depth]()
        computes[j]()


class Ring:
    def __init__(self, k, n, shape, dtype, name):
        self.bufs = [k.sb(shape, dtype) for _ in range(n)]
        self.res = [Res("%s%d" % (name, i)) for i in range(n)]
        self.i = 0
        self.n = n

    def next(self):
        i = self.i
        self.i = (i + 1) % self.n
        return self.bufs[i], self.res[i]


def rows_p(ap, p=128):
    return ap.rearrange("(c p) n -> p c n", p=p)


class Prog:
    def __init__(self, debug=False):
        self.debug = debug
        self.nc = nc = bass.Bass("TRN2", target_bir_lowering=False)
        self.inp = {}
        self.dbg_outs = {}

    def ext(self, name, shape, dtype=F32):
        t = self.nc.dram_tensor(name, list(shape), dtype, kind="ExternalInput").ap()
        self.inp[name] = (tuple(shape), dtype)
        return t

    def dram(self, name, shape, dtype):
        return self.nc.dram_tensor(name, list(shape), dtype).ap()

    def build(self, nlayers=2, stop_after=None, need=None, nblk=NBLK):
        nc = self.nc
        P = self
        x_in = P.ext("x", [NBLK * T, D])
        out_t = nc.dram_tensor("out", [NBLK * T, D], F32, kind="ExternalOutput").ap()
        ident_in = P.ext("ident", [128, 128])
        dftab = P.ext("dftab", [NBLK * S, 2048], BF16)
        cdft = P.ext("cdft", [256, 512], BF16)
        invcnt = P.ext("invcnt", [NBLK * 4 * T])
        L = []
        for i in range(nlayers):
            p = "l%d_" % i
            d = {}

            def wext(nm, shape, p=p, d=d):
                if need is None or nm in need:
                    d[nm] = P.ext(p + nm, shape)
            d["norm1"] = P.ext(p + "norm1", [D])
            d["norm2"] = P.ext(p + "norm2", [D])
            wext("w_in", [3072, 8192])
            wext("w_gate", [8192, 8192])
            wext("w_branch", [2048, 8192])
            wext("w_out", [2048, 8192])
            d["conv_wT"] = P.ext(p + "conv_wT", [BW, 31])
            d["chanp"] = P.ext(p + "chanp", [128, 32])
            d["pool_w"] = P.ext(p + "pool_w", [4 * 256, 256])
            d["gmlp_ln_g"] = P.ext(p + "gmlp_ln_g", [BW])
            d["gmlp_ln_b"] = P.ext(p + "gmlp_ln_b", [BW])
            d["gmlp_wsT"] = P.ext(p + "gmlp_wsT", [128, 8 * 128])
            d["gmlp_b"] = P.ext(p + "gmlp_b", [8 * 128])
            if i % 2 == 0:
                wext("ffn_w1", [5504, 8192])
                wext("ffn_w3", [5504, 8192])
                wext("ffn_w2", [2048, 22016])
            else:
                d["routerT"] = P.ext(p + "routerT", [NE * D])
                wext("exp_w1", [NE * 16 * 128, 32 * 256])
                wext("exp_w3", [NE * 16 * 128, 32 * 256])
                wext("exp_w2", [NE * 16 * 128, 32 * 256])
            L.append(d)
        final_norm = P.ext("final_norm", [D])

        B_ = range(nblk)
        h = [P.dram("h%d" % q, [T, D], F32) for q in B_]
        xnT = [P.dram("xnT%d" % q, [D, T], BF16) for q in B_]
        aT = [P.dram("aT%d" % q, [BW, T], F32) for q in B_]
        pT = [P.dram("pT%d" % q, [BW, T], F32) for q in B_]
        fT = P.dram("fT", [BW, T], BF16)
        uT = [P.dram("uT%d" % q, [BW, T], F32) for q in B_]
        vn = [P.dram("vn%d" % q, [T, BW], BF16) for q in B_]
        edges_g = P.dram("edges_g", [NBLK * BW, 64], F32)
        xcs_g = P.dram("xcs_g", [S, 2048], BF16)
        brT = P.dram("brT", [4 * BW, T], BF16)
        mergedT = P.dram("mergedT", [D, T], BF16)
        GT = P.dram("GT", [D, T], BF16)
        cmbT = P.dram("cmbT", [NE, T], F32)

        with ExitStack() as st:
            k = KB(nc, st)
            self.k = k
            identf = k.sb([128], F32, persist=True)
            identb = k.sb([128], BF16, persist=True)
            onesf = k.sb([128], F32, persist=True)
            epsb = k.sb([1], F32, persist=True)
            r_c = Res("consts", persist=True)
            self.identf, self.identb, self.onesf, self.epsb = identf, identb, onesf, epsb
            k.dma("sp", identf, ident_in, writes=[r_c])
            k.op("dve", lambda e: e.tensor_copy(out=identb, in_=identf), reads=[r_c], writes=[r_c])
            k.op("dve", lambda e: e.memset(onesf, 1.0), writes=[r_c])
            k.op("dve", lambda e: e.memset(epsb, EPS), writes=[r_c])
            k.barrier()

            W = [dict() for _ in range(nlayers)]

            def prep(li, name, kind):
                if name not in L[li]:
                    W[li][name] = None
                    return
                src = L[li][name]
                rows, cols = src.shape
                full = P.dram("wf_%d_%s" % (li, name), [rows, cols], BF16)
                r_full = Res("wf_%d_%s" % (li, name), persist=True)
                step = max(128, (8 * 1024 * 1024 // cols) // 128 * 128)
                r0 = 0
                first = True
                while r0 < rows:
                    r1 = min(rows, r0 + step)
                    k.dma("pool", full[r0:r1, :], src[r0:r1, :], writes=[r_full], join=not first)
                    first = False
                    r0 = r1
                W[li][name] = {"kind": kind, "full": full, "res": r_full}

            for li in range(nlayers):
                for name in ("w_in", "w_gate", "w_branch", "w_out"):
                    prep(li, name, "row")
                if li % 2 == 0:
                    for name in ("ffn_w1", "ffn_w3", "ffn_w2"):
                        prep(li, name, "row")
                else:
                    for name in ("exp_w1", "exp_w3", "exp_w2"):
                        prep(li, name, "exp")
                for nm in ("w_in", "w_gate", "w_branch", "w_out", "ffn_w1", "ffn_w3", "ffn_w2", "exp_w1", "exp_w3", "exp_w2"):
                    W[li].setdefault(nm, None)

            self.stopped = False

            def stop(tag):
                if stop_after == tag:
                    self.stopped = True
                return self.stopped

            h_src = [x_in[q * T:(q + 1) * T, :] for q in B_]
            for li in range(nlayers):
                if self.stopped:
                    break
                d = L[li]
                w = W[li]
                for q in B_:
                    self.ph_norm(h_src[q], d["norm1"], xnT[q])
                    if stop("n1"): break
                    self.ph_inproj(w["w_in"], xnT[q], aT[q], pT[q], fT, uT[q], edges_g[q * BW:(q + 1) * BW, :])
                    if stop("p2a"): break
                    self.ph_vproj(w["w_in"], xnT[q], vn[q], d["gmlp_ln_g"], d["gmlp_ln_b"])
                    self.ph_four1(fT, cdft, xcs_g[q * T:(q + 1) * T, :])
                if self.stopped: break
                if stop("p3a"): break
                for q in B_:
                    eL = edges_g[(q - 1) * BW:q * BW, :] if q > 0 else None
                    eR = edges_g[(q + 1) * BW:(q + 2) * BW, :] if q < nblk - 1 else None
                    self.ph_conv(aT[q], eL, eR, d["conv_wT"], d["chanp"], brT)
                    self.ph_pool(pT[q], eL, eR, invcnt[q * 4 * T:(q + 1) * 4 * T], d["pool_w"], d["chanp"], brT)
                    self.ph_gmlp(vn[q], uT[q], d["gmlp_wsT"], d["gmlp_b"], brT)
                    self.ph_four2(xcs_g, dftab[q * S:(q + 1) * S, :], brT)
                    if stop("p3"): break
                    self.ph_merge(w["w_gate"], w["w_branch"], xnT[q], brT, mergedT)
                    if stop("p4"): break
                    self.ph_outproj(w["w_out"], 0, 32, mergedT, h_src[q], h[q])
                    if stop("p5"): break
                    self.ph_norm(h[q], d["norm2"], xnT[q])
                    if li % 2 == 0:
                        c0 = 0
                        for nch in (32, 32, 22):
                            self.ph_ffn1(w["ffn_w1"], w["ffn_w3"], c0, nch, xnT[q], GT, None)
                            self.ph_outproj(w["ffn_w2"], c0, nch, GT, h[q], h[q])
                            c0 += nch
                    else:
                        self.ph_router(h[q], d["norm2"], d["routerT"], cmbT)
                        for e in range(NE):
                            self.ph_ffn1(w["exp_w1"], w["exp_w3"], 0, 32, xnT[q], GT, cmbT[e, :], wrow0=e * D)
                            self.ph_outproj(w["exp_w2"], e * 32, 32, GT, h[q], h[q])
                    if stop("l%d" % li): break
                h_src = h
            if not self.stopped:
                for q in B_:
                    self.ph_final(h[q], final_norm, out_t[q * T:(q + 1) * T, :])
            else:
                import os as _os
                want = _os.environ.get("DUMP", "xnT,aT,pT,uT,vn,brT").split(",")
                rr = None
                for name, ap in (("h", h[0]), ("xnT", xnT[0]), ("aT", aT[0]), ("pT", pT[0]), ("fT", fT), ("uT", uT[0]), ("vn", vn[0]),
                                 ("brT", brT), ("mergedT", mergedT), ("xcs_g", xcs_g), ("edges_g", edges_g), ("GT", GT), ("cmbT", cmbT)):
                    if name not in want:
                        continue
                    o = nc.dram_tensor("dbg_" + name, list(ap.shape), ap.dtype, kind="ExternalOutput").ap()
                    self.dbg_outs[name] = o
                    rr = Res("dbgcopy")
                    k.dma("sp", o, ap, writes=[rr])
                k.barrier()
            k.finish()
        return nc

    def _rstd(self, ss, n, tag):
        k = self.k
        sd = k.sb([n], F32)
        rstd = k.sb([n], F32)
        r_sd = Res("sd" + tag)
        r_rstd = Res("rstd" + tag)
        return sd, rstd, r_sd, r_rstd

    def ph_norm(self, h_src, gvec, xT_dst):
        k = self.k
        gbc = k.sb([D], F32); r_g = Res("gbc")
        k.dma("sp", gbc, gvec.partition_broadcast(128), writes=[r_g])
        xT = k.sb([32, T], BF16)
        r_xT = [Res("xT%d" % i) for i in range(8)]
        hring = Ring(k, 2, [D], F32, "ht")
        hsring = Ring(k, 2, [D], BF16, "hs")
        junk = k.sb([D], BF16); r_junk = Res("junk")
        ss = k.sb([8], F32); sd = k.sb([8], F32); rstd = k.sb([8], F32)
        r_ss = [Res("ss%d" % i) for i in range(8)]
        r_sd = [Res("sd%d" % i) for i in range(8)]
        r_rs = [Res("rs%d" % i) for i in range(8)]
        tiles = {}

        def load(tt):
            ht, r_ht = hring.next()
            tiles[tt] = (ht, r_ht)
            k.dma("sp", ht, h_src[tt * 128:(tt + 1) * 128, :], writes=[r_ht])

        def comp(tt):
            ht, r_ht = tiles[tt]
            hs, r_hs = hsring.next()
            k.op("act", lambda e: e.activation(out=junk, in_=ht, func=AF.Square, accum_out=ss[:, tt:tt + 1]),
                 reads=[r_ht], writes=[r_junk, r_ss[tt]])
            k.op("act", lambda e: e.activation(out=sd[:, tt:tt + 1], in_=ss[:, tt:tt + 1], func=AF.Sqrt, bias=self.epsb[:, 0:1], scale=1.0 / D),
                 reads=[r_ss[tt]], writes=[r_sd[tt]])
            k.op("dve", lambda e: e.reciprocal(out=rstd[:, tt:tt + 1], in_=sd[:, tt:tt + 1]), reads=[r_sd[tt]], writes=[r_rs[tt]])
            k.op("dve", lambda e: e.scalar_tensor_tensor(out=hs, in0=ht, scalar=rstd[:, tt:tt + 1], in1=gbc, op0=ALU.mult, op1=ALU.mult),
                 reads=[r_ht, r_rs[tt], r_g], writes=[r_hs])
            for j in range(4):
                bank = (tt % 2) * 4 + j
                pt = k.ps(bank, BF16)
                for f in range(8):
                    fc = j * 8 + f
                    k.op("pe", lambda e, f=f, fc=fc, pt=pt: e.transpose(pt[:, f * 128:(f + 1) * 128], hs[:, fc * 128:(fc + 1) * 128], self.identb),
                         reads=[r_hs], writes=[k.psres[bank]], signal=(f == 7))
                eng = "act" if j % 2 == 0 else "dve"
                dst = xT[:, j * 8:(j + 1) * 8, tt * 128:(tt + 1) * 128]
                srcv = pt.rearrange("p (a b) -> p a b", b=128)
                if eng == "act":
                    k.op("act", lambda e, dst=dst, srcv=srcv: e.activation(out=dst, in_=srcv, func=AF.Copy), reads=[k.psres[bank]], writes=[r_xT[tt]])
                else:
                    k.op("dve", lambda e, dst=dst, srcv=srcv: e.tensor_copy(out=dst, in_=srcv), reads=[k.psres[bank]], writes=[r_xT[tt]])

        pipeline([lambda tt=tt: load(tt) for tt in range(8)], [lambda tt=tt: comp(tt) for tt in range(8)], depth=1)
        k.dma("sp", rows_p(xT_dst), xT, reads=r_xT)
        k.barrier()

    def ph_final(self, h_src, gvec, out_t):
        k = self.k
        gbc = k.sb([D], F32); r_g = Res("gbc")
        k.dma("sp", gbc, gvec.partition_broadcast(128), writes=[r_g])
        hring = Ring(k, 2, [D], F32, "ht")
        oring = Ring(k, 2, [D], F32, "ot")
        junk = k.sb([D], BF16); r_junk = Res("junk")
        ss = k.sb([8], F32); sd = k.sb([8], F32); rstd = k.sb([8], F32)
        r_ss = [Res("ss%d" % i) for i in range(8)]
        r_sd = [Res("sd%d" % i) for i in range(8)]
        r_rs = [Res("rs%d" % i) for i in range(8)]
        for tt in range(8):
            ht, r_ht = hring.next()
            ot, r_ot = oring.next()
            k.dma("sp", ht, h_src[tt * 128:(tt + 1) * 128, :], writes=[r_ht])
            k.op("act", lambda e, ht=ht, tt=tt: e.activation(out=junk, in_=ht, func=AF.Square, accum_out=ss[:, tt:tt + 1]),
                 reads=[r_ht], writes=[r_junk, r_ss[tt]])
            k.op("act", lambda e, tt=tt: e.activation(out=sd[:, tt:tt + 1], in_=ss[:, tt:tt + 1], func=AF.Sqrt, bias=self.epsb[:, 0:1], scale=1.0 / D),
                 reads=[r_ss[tt]], writes=[r_sd[tt]])
            k.op("dve", lambda e, tt=tt: e.reciprocal(out=rstd[:, tt:tt + 1], in_=sd[:, tt:tt + 1]), reads=[r_sd[tt]], writes=[r_rs[tt]])
            k.op("dve", lambda e, ht=ht, ot=ot, tt=tt: e.scalar_tensor_tensor(out=ot, in0=ht, scalar=rstd[:, tt:tt + 1], in1=gbc, op0=ALU.mult, op1=ALU.mult),
                 reads=[r_ht, r_rs[tt], r_g], writes=[r_ot])
            k.dma("sp", out_t[tt * 128:(tt + 1) * 128, :], ot, reads=[r_ot])
        k.barrier()

    def _load_xT(self, src, nch=32, name="xT"):
        k = self.k
        xT = k.sb([nch, T], BF16)
        ng = (nch + 7) // 8
        rs = XRes([Res("%s%d" % (name, g)) for g in range(ng)])

        def issue(g):
            c1 = min(nch, (g + 1) * 8)
            k.dma("sp", xT[:, g * 8:c1, :], rows_p(src[g * 1024:c1 * 128, :]), writes=[rs.parts[g]])
        issue(0)
        rs.rest = lambda: [issue(g) for g in range(1, ng)]
        return xT, rs

    def _wtile(self, ring, wh, row0, nk, col0, ncols):
        k = self.k
        assert ncols == 256 and col0 % 256 == 0 and row0 % 128 == 0
        buf, r = ring.next()
        v = buf[:, 0:nk, 0:256]
        ct = col0 // 256
        if wh["kind"] == "exp":
            ct += (row0 // D) * 16
            row0 = row0 % D
        kc0 = row0 // 128
        src = wh["full"][ct * 128:(ct + 1) * 128, kc0 * 256:(kc0 + nk) * 256].rearrange("p (k n) -> p k n", n=256)
        k.dma("sp", v, src, reads=[wh["res"]], writes=[r])
        return v, r

    def ph_inproj(self, w_in, xnT, aT, pT, fT, uT, edges):
        k = self.k
        wf = w_in
        xT, r_x = self._load_xT(xnT)
        wring = Ring(k, 4, [32, 256], BF16, "w")
        sig_ring = Ring(k, 2, [512], F32, "sig")
        st_ring = Ring(k, 3, [2, T], F32, "stg")
        bank_ctr = [0]

        def nb():
            b = bank_ctr[0]
            bank_ctr[0] = (b + 1) % 8
            return b

        jobs = []
        for i in range(4):
            jobs.append(("glu", i))
        for kind, base in (("p", 2048), ("f", 3072), ("u", 4096)):
            for i in range(4):
                jobs.append((kind, base + i * 256, i))
        tiles = {}

        def load(j):
            job = jobs[j]
            if job[0] == "glu":
                i = job[1]
                t1 = self._wtile(wring, wf, 0, 32, i * 256, 256)
                t2 = self._wtile(wring, wf, 0, 32, 1024 + i * 256, 256)
                tiles[j] = (t1, t2)
            else:
                tiles[j] = (self._wtile(wring, wf, 0, 32, job[1], 256),)

        def comp(j):
            job = jobs[j]
            stg, r_stg = st_ring.next()
            if job[0] == "glu":
                i = job[1]
                (wl, r_wl), (wg, r_wg) = tiles[j]
                for oc in range(2):
                    for tt in range(2):
                        bl, bg = nb(), nb()
                        k.mm_group(k.ps(bl), bl, 32, lambda kc, oc=oc: wl[:, kc, oc * 128:(oc + 1) * 128],
                                   lambda kc, tt=tt: xT[:, kc, tt * 512:(tt + 1) * 512], lambda kc: [r_wl, r_x.kc(kc)])
                        k.mm_group(k.ps(bg), bg, 32, lambda kc, oc=oc: wg[:, kc, oc * 128:(oc + 1) * 128],
                                   lambda kc, tt=tt: xT[:, kc, tt * 512:(tt + 1) * 512], lambda kc: [r_wg, r_x.kc(kc)])
                        sg, r_sg = sig_ring.next()
                        k.op("act", lambda e, sg=sg, bg=bg: e.activation(out=sg, in_=k.ps(bg), func=AF.Sigmoid), reads=[k.psres[bg]], writes=[r_sg])
                        k.op("dve", lambda e, sg=sg, bl=bl, oc=oc, tt=tt: e.tensor_tensor(out=stg[:, oc, tt * 512:(tt + 1) * 512], in0=sg, in1=k.ps(bl), op=ALU.mult),
                             reads=[r_sg, k.psres[bl]], writes=[r_stg])
                rows = slice(i * 256, (i + 1) * 256)
                k.dma("sp", rows_p(aT[rows, :]), stg, reads=[r_stg])
                k.dma("sp", rows_p(edges[rows, 0:15]), stg[:, :, 0:15], reads=[r_stg])
                k.dma("sp", rows_p(edges[rows, 16:31]), stg[:, :, T - 15:T], reads=[r_stg])
            else:
                kind, col0, i = job
                (wt, r_wt), = tiles[j]
                if kind == "f":
                    stgb = stg.bitcast(BF16)[:, :, 0:T] if False else None
                for oc in range(2):
                    for tt in range(2):
                        b = nb()
                        k.mm_group(k.ps(b), b, 32, lambda kc, oc=oc: wt[:, kc, oc * 128:(oc + 1) * 128],
                                   lambda kc, tt=tt: xT[:, kc, tt * 512:(tt + 1) * 512], lambda kc, r_wt=r_wt: [r_wt, r_x.kc(kc)])
                        if kind == "f":
                            dst = self._bfview(stg)[:, oc, tt * 512:(tt + 1) * 512]
                        else:
                            dst = stg[:, oc, tt * 512:(tt + 1) * 512]
                        if (oc + tt) % 2 == 0:
                            k.op("act", lambda e, dst=dst, b=b: e.activation(out=dst, in_=k.ps(b), func=AF.Copy), reads=[k.psres[b]], writes=[r_stg])
                        else:
                            k.op("dve", lambda e, dst=dst, b=b: e.tensor_copy(out=dst, in_=k.ps(b)), reads=[k.psres[b]], writes=[r_stg])
                rows = slice(i * 256, (i + 1) * 256)
                if kind == "f":
                    k.dma("sp", rows_p(fT[rows, :]), self._bfview(stg)[:, :, 0:T], reads=[r_stg])
                elif kind == "u":
                    k.dma("sp", rows_p(uT[rows, :]), stg, reads=[r_stg])
                else:
                    k.dma("sp", rows_p(pT[rows, :]), stg, reads=[r_stg])
                    k.dma("sp", rows_p(edges[rows, 32:40]), stg[:, :, 0:8], reads=[r_stg])
                    k.dma("sp", rows_p(edges[rows, 40:48]), stg[:, :, T - 8:T], reads=[r_stg])

        pipeline([lambda j=j: load(j) for j in range(len(jobs))], [lambda j=j: comp(j) for j in range(len(jobs))], depth=1, after_first=r_x.rest)
        k.barrier()

    @staticmethod
    def _bfview(stg):
        return stg.bitcast(BF16)

    def ph_vproj(self, w_in, xnT, vn_dst, ln_g, ln_b):
        k = self.k
        wf = w_in
        xT, r_x = self._load_xT(xnT)
        wring = Ring(k, 3, [32, 256], BF16, "w")
        v = k.sb([8, BW], F32)
        r_v = [Res("v%d" % i) for i in range(4)]
        gb = k.sb([BW], F32); bb = k.sb([BW], F32); r_gb = Res("gb")
        k.dma("sp", gb, ln_g.partition_broadcast(128), writes=[r_gb])
        r_bb = Res("bb")
        k.dma("sp", bb, ln_b.partition_broadcast(128), writes=[r_bb])
        tiles = {}
        bank_ctr = [0]

        def load(ct):
            tiles[ct] = self._wtile(wring, wf, 0, 32, 5120 + ct * 256, 256)

        def comp(ct):
            wt, r_wt = tiles[ct]
            for tt in range(8):
                b = bank_ctr[0]
                bank_ctr[0] = (b + 1) % 8
                out = k.ps(b)[:, 0:256]
                k.mm_group(out, b, 32, lambda kc, tt=tt: xT[:, kc, tt * 128:(tt + 1) * 128], lambda kc: wt[:, kc, :], lambda kc, r_wt=r_wt: [r_wt, r_x.kc(kc)])
                dst = v[:, tt, ct * 256:(ct + 1) * 256]
                if tt % 2 == 0:
                    k.op("act", lambda e, dst=dst, out=out: e.activation(out=dst, in_=out, func=AF.Copy), reads=[k.psres[b]], writes=[r_v[ct]])
                else:
                    k.op("dve", lambda e, dst=dst, out=out: e.tensor_copy(out=dst, in_=out), reads=[k.psres[b]], writes=[r_v[ct]])

        pipeline([lambda c=c: load(c) for c in range(4)], [lambda c=c: comp(c) for c in range(4)], depth=2, after_first=r_x.rest)
        s1 = k.sb([8], F32); s2 = k.sb([8], F32); mean = k.sb([8], F32); msq = k.sb([8], F32)
        var = k.sb([8], F32); sd = k.sb([8], F32); rstd = k.sb([8], F32)
        sq = k.sb([8, BW], F32)
        r_s = Res("stats"); r_sq = Res("sq")
        k.op("dve", lambda e: e.reduce_sum(out=s1, in_=v, axis=AX.X), reads=r_v, writes=[r_s])
        k.op("dve", lambda e: e.tensor_tensor(out=sq, in0=v, in1=v, op=ALU.mult), reads=r_v, writes=[r_sq])
        k.op("dve", lambda e: e.reduce_sum(out=s2, in_=sq, axis=AX.X), reads=[r_sq], writes=[r_s])
        k.op("dve", lambda e: e.tensor_scalar(out=mean, in0=s1, scalar1=1.0 / BW, scalar2=None, op0=ALU.mult), reads=[r_s], writes=[r_s])
        k.op("dve", lambda e: e.tensor_tensor(out=msq, in0=mean, in1=mean, op=ALU.mult), reads=[r_s], writes=[r_s])
        k.op("dve", lambda e: e.scalar_tensor_tensor(out=var, in0=s2, scalar=1.0 / BW, in1=msq, op0=ALU.mult, op1=ALU.subtract), reads=[r_s], writes=[r_s])
        k.op("act", lambda e: e.activation(out=sd, in_=var, func=AF.Sqrt, bias=self.epsb[:, 0:1], scale=1.0), reads=[r_s], writes=[r_s])
        k.op("dve", lambda e: e.reciprocal(out=rstd, in_=sd), reads=[r_s], writes=[r_s])
        for tt in range(8):
            k.op("dve", lambda e, tt=tt: e.tensor_scalar(out=sq[:, tt, :], in0=v[:, tt, :], scalar1=mean[:, tt:tt + 1], scalar2=rstd[:, tt:tt + 1],
                                                          op0=ALU.subtract, op1=ALU.mult), reads=r_v + [r_s], writes=[r_sq])
        k.op("dve", lambda e: e.tensor_tensor(out=sq, in0=sq, in1=gb.unsqueeze(1).to_broadcast([128, 8, BW]), op=ALU.mult), reads=[r_sq, r_gb], writes=[r_sq])
        vb = k.sb([8, BW], BF16); r_vb = Res("vb")
        k.op("dve", lambda e: e.tensor_tensor(out=vb, in0=sq, in1=bb.unsqueeze(1).to_broadcast([128, 8, BW]), op=ALU.add), reads=[r_sq, r_bb], writes=[r_vb])
        k.dma("sp", vn_dst.rearrange("(t p) c -> p t c", p=128), vb, reads=[r_vb])
        k.barrier()

    def ph_four1(self, fT, cdft, xcs):
        k = self.k
        f, r_f = self._load_xT(fT, 8, "fT")
        r_f.rest()
        cd = k.sb([2, 512], BF16); r_cd = Res("cd")
        k.dma("sp", cd, rows_p(cdft), writes=[r_cd])
        oring = Ring(k, 2, [4, 512], BF16, "xo")
        bctr = 0
        for n in range(8):
            xo, r_xo = oring.next()
            for g in range(4):
                b = bctr
                bctr = (bctr + 1) % 8
                k.mm_group(k.ps(b), b, 2, lambda cc, g=g, n=n: f[:, 2 * g + cc, n * 128:(n + 1) * 128], lambda cc: cd[:, cc, :], [r_f.parts[0], r_cd])
                if g % 2 == 0:
                    k.op("act", lambda e, xo=xo, g=g, b=b: e.activation(out=xo[:, g, :], in_=k.ps(b), func=AF.Copy), reads=[k.psres[b]], writes=[r_xo])
                else:
                    k.op("dve", lambda e, xo=xo, g=g, b=b: e.tensor_copy(out=xo[:, g, :], in_=k.ps(b)), reads=[k.psres[b]], writes=[r_xo])
            k.dma("sp", xcs[n * 128:(n + 1) * 128, :].rearrange("p (g c) -> p g c", g=4), xo, reads=[r_xo])
        k.barrier()

    def _halo_load(self, dst, src_rows, cols, r_dst):
        k = self.k
        if src_rows is None:
            k.op("dve", lambda e: e.memset(dst, 0.0), writes=[r_dst])
        else:
            k.dma("sp", dst, rows_p(src_rows[:, cols]), writes=[r_dst])

    def ph_conv(self, aT, eL, eR, conv_wT, chanp, brT):
        k = self.k
        HW = 15
        aH = k.sb([8, T + 2 * HW], F32); r_a = Res("aH"); r_aL = Res("aHL"); r_aR = Res("aHR")
        k.dma("sp", aH[:, :, HW:HW + T], rows_p(aT), writes=[r_a])
        self._halo_load(aH[:, :, 0:HW], eL, slice(16, 31), r_aL)
        self._halo_load(aH[:, :, HW + T:HW + T + HW], eR, slice(0, 15), r_aR)
        cw = k.sb([8, 31], F32); r_cw = Res("cw")
        k.dma("sp", cw, rows_p(conv_wT), writes=[r_cw])
        cp = k.sb([32], F32); r_cp = Res("cp")
        k.dma("sp", cp, chanp, writes=[r_cp])
        acc = k.sb([8, T], F32)
        r_acc = [Res("acc%d" % c) for c in range(8)]
        for c in range(8):
            for j in range(31):
                src = aH[:, c, j:j + T]
                if j == 0:
                    k.op("dve", lambda e, c=c, src=src: e.tensor_scalar(out=acc[:, c, :], in0=src, scalar1=cw[:, c, 0:1], scalar2=cp[:, c:c + 1], op0=ALU.mult, op1=ALU.add),
                         reads=[r_a, r_aL, r_aR, r_cw, r_cp], writes=[r_acc[c]])
                else:
                    k.op("dve", lambda e, c=c, j=j, src=src: e.scalar_tensor_tensor(out=acc[:, c, :], in0=src, scalar=cw[:, c, j:j + 1], in1=acc[:, c, :], op0=ALU.mult, op1=ALU.add),
                         reads=[r_a, r_aL, r_aR, r_cw], writes=[r_acc[c]])
        sq = k.sb([8, T], F32); r_sq = [Res("sq%d" % c) for c in range(8)]
        for c in range(8):
            k.op("act", lambda e, c=c: e.activation(out=sq[:, c, :], in_=acc[:, c, :], func=AF.Square), reads=[r_acc[c]], writes=[r_sq[c]])
        mean = k.sb([T], F32); msq = k.sb([T], F32); var = k.sb([T], F32); sd = k.sb([T], F32); rstd = k.sb([T], F32)
        r_st = Res("st")
        for tt in range(2):
            b1, b2 = tt * 2, tt * 2 + 1
            k.mm_group(k.ps(b1), b1, 8, lambda c: self.onesf, lambda c, tt=tt: acc[:, c, tt * 512:(tt + 1) * 512], r_acc)
            k.mm_group(k.ps(b2), b2, 8, lambda c: self.onesf, lambda c, tt=tt: sq[:, c, tt * 512:(tt + 1) * 512], r_sq)
            sl = slice(tt * 512, (tt + 1) * 512)
            k.op("act", lambda e, sl=sl, b1=b1: e.activation(out=mean[:, sl], in_=k.ps(b1), func=AF.Copy, scale=1.0 / BW), reads=[k.psres[b1]], writes=[r_st])
            k.op("dve", lambda e, sl=sl: e.tensor_tensor(out=msq[:, sl], in0=mean[:, sl], in1=mean[:, sl], op=ALU.mult), reads=[r_st], writes=[r_st])
            k.op("dve", lambda e, sl=sl, b2=b2: e.scalar_tensor_tensor(out=var[:, sl], in0=k.ps(b2), scalar=1.0 / BW, in1=msq[:, sl], op0=ALU.mult, op1=ALU.subtract),
                 reads=[k.psres[b2], r_st], writes=[r_st])
            k.op("act", lambda e, sl=sl: e.activation(out=sd[:, sl], in_=var[:, sl], func=AF.Sqrt, bias=self.epsb[:, 0:1], scale=1.0), reads=[r_st], writes=[r_st])
            k.op("dve", lambda e, sl=sl: e.reciprocal(out=rstd[:, sl], in_=sd[:, sl]), reads=[r_st], writes=[r_st])
        oring = Ring(k, 2, [T], BF16, "ya")
        for c in range(8):
            k.op("dve", lambda e, c=c: e.tensor_tensor(out=sq[:, c, :], in0=acc[:, c, :], in1=mean, op=ALU.subtract), reads=[r_acc[c], r_st, r_sq[c]], writes=[r_sq[c]])
            k.op("dve", lambda e, c=c: e.tensor_tensor(out=sq[:, c, :], in0=sq[:, c, :], in1=rstd, op=ALU.mult), reads=[r_st], writes=[r_sq[c]])
            ya, r_ya = oring.next()
            k.op("act", lambda e, c=c, ya=ya: e.activation(out=ya, in_=sq[:, c, :], func=AF.Silu, scale=cp[:, 8 + c:9 + c], bias=cp[:, 16 + c:17 + c]),
                 reads=[r_sq[c], r_cp], writes=[r_ya])
            k.dma("sp", brT[c * 128:(c + 1) * 128, :], ya, reads=[r_ya])
        k.barrier()

    def ph_pool(self, pT, eL, eR, invcnt, pool_w, chanp, brT):
        k = self.k
        HW = 8
        pH = k.sb([8, T + 2 * HW], F32); r_p = Res("pH"); r_pL = Res("pHL"); r_pR = Res("pHR")
        k.dma("sp", pH[:, :, HW:HW + T], rows_p(pT), writes=[r_p])
        self._halo_load(pH[:, :, 0:HW], eL, slice(40, 48), r_pL)
        self._halo_load(pH[:, :, HW + T:HW + T + HW], eR, slice(32, 40), r_pR)
        r_pj = Res("pHj")
        k.op("dve", lambda e: e.tensor_copy(out=pH[:, 0, 0:1], in_=pH[:, 0, 0:1]), reads=[r_pL, r_pR, r_p], writes=[r_p])
        icbf = k.sb([4 * T], F32); r_ic = Res("icb")
        k.dma("sp", icbf, invcnt.partition_broadcast(128), writes=[r_ic])
        icb = icbf.rearrange("p (g t) -> p g t", g=4)
        cp = k.sb([32], F32); r_cp = Res("cp")
        k.dma("sp", cp, chanp, writes=[r_cp])
        pwf = k.sb([8, 256], F32); r_pwf = Res("pwf")
        k.dma("sp", pwf, rows_p(pool_w), writes=[r_pwf])
        pw = k.sb([8, 256], BF16); r_pw = Res("pw")
        k.op("act", lambda e: e.activation(out=pw, in_=pwf, func=AF.Copy), reads=[r_pwf], writes=[r_pw])
        A = k.sb([2, T + 16], F32); B = k.sb([2, T + 16], F32); r_A = Res("A"); r_B = Res("B")
        pooled_ring = Ring(k, 2, [2, T], BF16, "pooled")
        oring = Ring(k, 2, [T], BF16, "yb")
        bctr = 0
        for g, w in enumerate((2, 4, 8, 16)):
            X = pH[:, 2 * g:2 * g + 2, :]
            L0 = T + 16
            cur, r_cur, ln = X, r_p, L0
            step = 1
            bufs = [(A, r_A), (B, r_B)]
            bi = 0
            while step < w:
                dst, r_dst = bufs[bi]
                bi ^= 1
                nl = ln - step
                k.op("dve", lambda e, dst=dst, cur=cur, nl=nl, step=step: e.tensor_tensor(out=dst[:, :, 0:nl], in0=cur[:, :, 0:nl], in1=cur[:, :, step:step + nl], op=ALU.add),
                     reads=[r_cur], writes=[r_dst])
                cur, r_cur, ln = dst, r_dst, nl
                step *= 2
            off = HW - w // 2
            dst, r_dst = bufs[bi]
            k.op("dve", lambda e, dst=dst, cur=cur, off=off, g=g: e.tensor_tensor(out=dst[:, :, 0:T], in0=cur[:, :, off:off + T],
                                                                                 in1=icb[:, g, :].unsqueeze(1).to_broadcast([128, 2, T]), op=ALU.mult),
                 reads=[r_cur, r_ic], writes=[r_dst])
            pooled, r_pl = pooled_ring.next()
            k.op("dve", lambda e, dst=dst, X=X, pooled=pooled: e.tensor_tensor(out=pooled, in0=dst[:, :, 0:T], in1=X[:, :, HW:HW + T], op=ALU.subtract),
                 reads=[r_dst, r_p], writes=[r_pl])
            for ec in range(2):
                yb, r_yb = oring.next()
                ch = 2 * g + ec
                for tt in range(2):
                    b = bctr
                    bctr = (bctr + 1) % 8
                    k.mm_group(k.ps(b), b, 2, lambda cc, g=g, ec=ec: pw[:, 2 * g + cc, ec * 128:(ec + 1) * 128],
                               lambda cc, tt=tt, pooled=pooled: pooled[:, cc, tt * 512:(tt + 1) * 512], [r_pw, r_pl])
                    k.op("act", lambda e, yb=yb, tt=tt, b=b, ch=ch: e.activation(out=yb[:, tt * 512:(tt + 1) * 512], in_=k.ps(b), func=AF.Copy, scale=cp[:, 24 + ch:25 + ch]),
                         reads=[k.psres[b], r_cp], writes=[r_yb])
                k.dma("sp", brT[BW + ch * 128:BW + (ch + 1) * 128, :], yb, reads=[r_yb])
        k.barrier()

    def ph_gmlp(self, vn, uT, wsT, gb_vec, brT):
        k = self.k
        v = k.sb([8, BW], BF16); r_v = Res("vn")
        k.dma("sp", v, vn.rearrange("(n q) c -> q n c", q=128), writes=[r_v])
        u = k.sb([8, T], F32); r_u = Res("u")
        k.dma("sp", u, rows_p(uT), writes=[r_u])
        wsf = k.sb([8 * 128], F32); r_wsf = Res("wsf")
        k.dma("sp", wsf, wsT, writes=[r_wsf])
        ws = k.sb([8, 128], BF16); r_ws = Res("ws")
        k.op("act", lambda e: e.activation(out=ws, in_=wsf.rearrange("q (h p) -> q h p", h=8), func=AF.Copy), reads=[r_wsf], writes=[r_ws])
        bb = k.sb([8, 128], F32); r_bb = Res("bb")
        k.dma("sp", bb.rearrange("p h q -> p (h q)"), gb_vec.partition_broadcast(128), writes=[r_bb])
        tring = Ring(k, 2, [512], F32, "tmp")
        oring = Ring(k, 2, [T], BF16, "yd")
        for h in range(8):
            yd, r_yd = oring.next()
            for j in range(2):
                b = (h % 4) * 2 + j
                for n4 in range(4):
                    n = j * 4 + n4
                    k.op("pe", lambda e, b=b, n4=n4, n=n, h=h: e.matmul(k.ps(b)[:, n4 * 128:(n4 + 1) * 128], v[:, n, h * 128:(h + 1) * 128], ws[:, h, :], start=True, stop=True),
                         reads=[r_v, r_ws], writes=[k.psres[b]], signal=(n4 == 3))
                tmp, r_tmp = tring.next()
                k.op("dve", lambda e, b=b, h=h, tmp=tmp: e.tensor_tensor(out=tmp.rearrange("p (n q) -> p n q", q=128), in0=k.ps(b).rearrange("p (n q) -> p n q", q=128),
                                                                       in1=bb[:, h, :].unsqueeze(1).to_broadcast([128, 4, 128]), op=ALU.add),
                     reads=[k.psres[b], r_bb], writes=[r_tmp])
                k.op("dve", lambda e, tmp=tmp, yd=yd, h=h, j=j: e.tensor_tensor(out=yd[:, j * 512:(j + 1) * 512], in0=tmp, in1=u[:, h, j * 512:(j + 1) * 512], op=ALU.mult),
                     reads=[r_tmp, r_u], writes=[r_yd])
            k.dma("sp", brT[3 * BW + h * 128:3 * BW + (h + 1) * 128, :], yd, reads=[r_yd])
        k.barrier()

    def ph_four2(self, xcs_g, dftab, brT):
        k = self.k
        xring = Ring(k, 2, [32, 512], BF16, "X")
        tring = Ring(k, 3, [4, 2048], BF16, "tab")
        oring = Ring(k, 2, [T], BF16, "yc")
        r_tab = Res("dftab_in")
        for g in range(4):
            X, r_X = xring.next()
            k.dma("sp", X, rows_p(xcs_g[:, g * 512:(g + 1) * 512]), writes=[r_X])
            banks = [(g % 2) * 4 + i for i in range(4)]
            tabs = {}

            def load(sb):
                tb, r_tb = tring.next()
                tabs[sb] = (tb, r_tb)
                k.dma("sp", tb, rows_p(dftab[sb * 512:(sb + 1) * 512, :]), writes=[r_tb])

            def comp(sb, X=X, r_X=r_X, banks=banks):
                tb, r_tb = tabs[sb]
                for sc in range(4):
                    s = sb * 4 + sc
                    for cs in range(2):
                        for cc in range(2):
                            for kt in range(2):
                                b = banks[cc * 2 + kt]
                                first = (s == 0 and cs == 0)
                                last = (s == 31 and cs == 1)
                                k.op("pe", lambda e, b=b, s=s, cs=cs, cc=cc, kt=kt, sc=sc, tb=tb, first=first, last=last:
                                     e.matmul(k.ps(b), X[:, s, cs * 256 + cc * 128:cs * 256 + (cc + 1) * 128], tb[:, sc, cs * 1024 + kt * 512:cs * 1024 + (kt + 1) * 512], start=first, stop=last),
                                     reads=[r_X, r_tb], writes=[k.psres[b]], signal=(last or (sc == 3 and cs == 1 and cc == 1 and kt == 1)))
            pipeline([lambda sb=sb: load(sb) for sb in range(8)], [lambda sb=sb: comp(sb) for sb in range(8)], depth=2)
            for cc in range(2):
                yc, r_yc = oring.next()
                for kt in range(2):
                    b = banks[cc * 2 + kt]
                    k.op("act", lambda e, yc=yc, kt=kt, b=b: e.activation(out=yc[:, kt * 512:(kt + 1) * 512], in_=k.ps(b), func=AF.Copy, scale=1.0 / 1024.0),
                         reads=[k.psres[b]], writes=[r_yc])
                row = 2 * BW + g * 256 + cc * 128
                k.dma("sp", brT[row:row + 128, :], yc, reads=[r_yc])
        k.barrier()

    def ph_merge(self, w_gate, w_branch, xnT, brT, mergedT):
        k = self.k
        wg_f = w_gate
        wb_f = w_branch
        xT, r_x = self._load_xT(xnT)
        gring = Ring(k, 2, [32, 256], BF16, "wg")
        bring = Ring(k, 2, [8, 256], BF16, "wb")
        brring = Ring(k, 2, [8, T], BF16, "br")
        accring = Ring(k, 2, [2, T], F32, "acc")
        sring = Ring(k, 2, [512], F32, "sig")
        tring = Ring(k, 2, [512], F32, "tmp")
        mring = Ring(k, 2, [2, T], BF16, "mo")
        jobs = [(dp, g) for dp in range(16) for g in range(4)]
        tiles = {}
        bctr = [0]

        def nb():
            b = bctr[0]
            bctr[0] = (b + 1) % 8
            return b

        def load(j):
            dp, g = jobs[j]
            t1 = self._wtile(gring, wg_f, 0, 32, g * D + dp * 256, 256)
            t2 = self._wtile(bring, wb_f, g * BW, 8, dp * 256, 256)
            br, r_br = brring.next()
            k.dma("sp", br, rows_p(brT[g * BW:(g + 1) * BW, :]), writes=[r_br])
            tiles[j] = (t1, t2, (br, r_br))

        cur = {}

        def comp(j):
            dp, g = jobs[j]
            (wg, r_wgt), (wb, r_wbt), (br, r_br) = tiles[j]
            if g == 0:
                cur["acc"] = accring.next()
            acc, r_acc = cur["acc"]
            for oc in range(2):
                for tt in range(2):
                    bg, bp = nb(), nb()
                    k.mm_group(k.ps(bg), bg, 32, lambda kc, oc=oc: wg[:, kc, oc * 128:(oc + 1) * 128],
                               lambda kc, tt=tt: xT[:, kc, tt * 512:(tt + 1) * 512], lambda kc, r_wgt=r_wgt: [r_wgt, r_x.kc(kc)])
                    k.mm_group(k.ps(bp), bp, 8, lambda kc, oc=oc: wb[:, kc, oc * 128:(oc + 1) * 128],
                               lambda kc, tt=tt: br[:, kc, tt * 512:(tt + 1) * 512], [r_wbt, r_br])
                    sg, r_sg = sring.next()
                    k.op("act", lambda e, sg=sg, bg=bg: e.activation(out=sg, in_=k.ps(bg), func=AF.Sigmoid), reads=[k.psres[bg]], writes=[r_sg])
                    dst = acc[:, oc, tt * 512:(tt + 1) * 512]
                    if g == 0:
                        k.op("dve", lambda e, sg=sg, bp=bp, dst=dst: e.tensor_tensor(out=dst, in0=sg, in1=k.ps(bp), op=ALU.mult),
                             reads=[r_sg, k.psres[bp]], writes=[r_acc])
                    else:
                        tmp, r_tmp = tring.next()
                        k.op("dve", lambda e, sg=sg, bp=bp, tmp=tmp: e.tensor_tensor(out=tmp, in0=sg, in1=k.ps(bp), op=ALU.mult),
                             reads=[r_sg, k.psres[bp]], writes=[r_tmp])
                        k.op("dve", lambda e, tmp=tmp, dst=dst: e.tensor_tensor(out=dst, in0=dst, in1=tmp, op=ALU.add),
                             reads=[r_tmp], writes=[r_acc])
            if g == 3:
                mo, r_mo = mring.next()
                k.op("act", lambda e, mo=mo, acc=acc: e.activation(out=mo, in_=acc, func=AF.Copy), reads=[r_acc], writes=[r_mo])
                k.dma("sp", rows_p(mergedT[dp * 256:(dp + 1) * 256, :]), mo, reads=[r_mo])

        pipeline([lambda j=j: load(j) for j in range(len(jobs))], [lambda j=j: comp(j) for j in range(len(jobs))], depth=1, after_first=r_x.rest)
        k.barrier()

    def ph_outproj(self, w2, kc0, nk, actT_src, h_src, h_dst):
        k = self.k
        wf = w2
        aT_, r_a = self._load_xT(actT_src, nk, "actT")
        wring = Ring(k, 3, [32, 256], BF16, "w")
        hin_ring = Ring(k, 3, [8, 256], F32, "hin")
        hout_ring = Ring(k, 2, [8, 256], F32, "hout")
        tiles = {}
        bctr = [0]

        def load(ct):
            wt = self._wtile(wring, wf, kc0 * 128, nk, ct * 256, 256)
            hin, r_hin = hin_ring.next()
            k.dma("sp", hin, h_src[:, ct * 256:(ct + 1) * 256].rearrange("(t p) c -> p t c", p=128), writes=[r_hin])
            tiles[ct] = (wt, (hin, r_hin))

        def comp(ct):
            (wt, r_wt), (hin, r_hin) = tiles[ct]
            hout, r_hout = hout_ring.next()
            for tt in range(8):
                b = bctr[0]
                bctr[0] = (b + 1) % 8
                out = k.ps(b)[:, 0:256]
                k.mm_group(out, b, nk, lambda kc, tt=tt: aT_[:, kc, tt * 128:(tt + 1) * 128], lambda kc: wt[:, kc, :], lambda kc, r_wt=r_wt: [r_wt, r_a.kc(kc)])
                k.op("dve", lambda e, out=out, tt=tt, hin=hin, hout=hout: e.tensor_tensor(out=hout[:, tt, :], in0=out, in1=hin[:, tt, :], op=ALU.add),
                     reads=[k.psres[b], r_hin], writes=[r_hout])
            k.dma("sp", h_dst[:, ct * 256:(ct + 1) * 256].rearrange("(t p) c -> p t c", p=128), hout, reads=[r_hout])

        pipeline([lambda c=c: load(c) for c in range(16)], [lambda c=c: comp(c) for c in range(16)], depth=2, after_first=r_a.rest)
        k.barrier()

    def ph_ffn1(self, w1, w3, c0, nch, xnT, GT, cmb_row, wrow0=0):
        k = self.k
        w1f = w1
        w3f = w3
        xT, r_x = self._load_xT(xnT)
        wring = Ring(k, 4, [32, 256], BF16, "w")
        sring = Ring(k, 2, [512], F32, "sil")
        tring = Ring(k, 2, [512], F32, "tmp")
        gring = Ring(k, 2, [2, T], BF16, "g")
        cb = None
        if cmb_row is not None:
            cb = k.sb([T], F32); r_cb = Res("cb")
            k.dma("sp", cb, cmb_row.partition_broadcast(128), writes=[r_cb])
        tiles = {}
        bctr = [0]

        def nb():
            b = bctr[0]
            bctr[0] = (b + 1) % 8
            return b

        def load(j):
            col = (c0 + 2 * j) * 128
            tiles[j] = (self._wtile(wring, w1f, wrow0, 32, col, 256), self._wtile(wring, w3f, wrow0, 32, col, 256))

        def comp(j):
            (wa, r_wa), (wb, r_wbt) = tiles[j]
            gt, r_gt = gring.next()
            for oc in range(2):
                for tt in range(2):
                    ba, bb_ = nb(), nb()
                    k.mm_group(k.ps(ba), ba, 32, lambda kc, oc=oc: wa[:, kc, oc * 128:(oc + 1) * 128],
                               lambda kc, tt=tt: xT[:, kc, tt * 512:(tt + 1) * 512], lambda kc, r_wa=r_wa: [r_wa, r_x.kc(kc)])
                    k.mm_group(k.ps(bb_), bb_, 32, lambda kc, oc=oc: wb[:, kc, oc * 128:(oc + 1) * 128],
                               lambda kc, tt=tt: xT[:, kc, tt * 512:(tt + 1) * 512], lambda kc, r_wbt=r_wbt: [r_wbt, r_x.kc(kc)])
                    sl, r_sl = sring.next()
                    k.op("act", lambda e, sl=sl, ba=ba: e.activation(out=sl, in_=k.ps(ba), func=AF.Silu), reads=[k.psres[ba]], writes=[r_sl])
                    dst = gt[:, oc, tt * 512:(tt + 1) * 512]
                    if cb is None:
                        k.op("dve", lambda e, sl=sl, bb_=bb_, dst=dst: e.tensor_tensor(out=dst, in0=sl, in1=k.ps(bb_), op=ALU.mult),
                             reads=[r_sl, k.psres[bb_]], writes=[r_gt])
                    else:
                        tmp, r_tmp = tring.next()
                        k.op("dve", lambda e, sl=sl, bb_=bb_, tmp=tmp: e.tensor_tensor(out=tmp, in0=sl, in1=k.ps(bb_), op=ALU.mult),
                             reads=[r_sl, k.psres[bb_]], writes=[r_tmp])
                        k.op("dve", lambda e, tmp=tmp, dst=dst, tt=tt: e.tensor_tensor(out=dst, in0=tmp, in1=cb[:, tt * 512:(tt + 1) * 512], op=ALU.mult),
                             reads=[r_tmp, r_cb], writes=[r_gt])
            k.dma("sp", rows_p(GT[j * 256:(j + 1) * 256, :]), gt, reads=[r_gt])

        nj = nch // 2
        pipeline([lambda j=j: load(j) for j in range(nj)], [lambda j=j: comp(j) for j in range(nj)], depth=1, after_first=r_x.rest)
        k.barrier()

    def ph_router(self, h_src, gvec, routerT, cmbT):
        k = self.k
        gbc = k.sb([D], F32); r_g = Res("gbc")
        k.dma("sp", gbc, gvec.partition_broadcast(128), writes=[r_g])
        gr = k.sb([NE, D], F32); r_gr = [Res("gr%d" % e_) for e_ in range(NE)]
        for e_ in range(NE):
            k.dma("sp", gr[:, e_, :], routerT[e_ * D:(e_ + 1) * D].partition_broadcast(128), writes=[r_gr[e_]])
            k.op("dve", lambda e, e_=e_: e.tensor_tensor(out=gr[:, e_, :], in0=gr[:, e_, :], in1=gbc, op=ALU.mult), reads=[r_g], writes=[r_gr[e_]])
        hring = Ring(k, 2, [D], F32, "ht")
        prod = k.sb([D], F32); r_prod = Res("prod")
        junk, r_junk = prod, r_prod
        ss = k.sb([8], F32); sd = k.sb([8], F32); rstd = k.sb([8], F32)
        r_ss = Res("ss")
        lg = k.sb([8, NE], F32); r_lg = Res("lg")
        for tt in range(8):
            ht, r_ht = hring.next()
            k.dma("sp", ht, h_src[tt * 128:(tt + 1) * 128, :], writes=[r_ht])
            k.op("act", lambda e, ht=ht, tt=tt: e.activation(out=junk, in_=ht, func=AF.Square, accum_out=ss[:, tt:tt + 1]), reads=[r_ht], writes=[r_junk, r_ss])
            for e_ in range(NE):
                k.op("dve", lambda e, ht=ht, e_=e_: e.tensor_tensor(out=prod, in0=ht, in1=gr[:, e_, :], op=ALU.mult), reads=[r_ht, r_gr[e_]], writes=[r_prod])
                k.op("dve", lambda e, tt=tt, e_=e_: e.reduce_sum(out=lg[:, tt, e_:e_ + 1], in_=prod, axis=AX.X), reads=[r_prod], writes=[r_lg])
        k.op("act", lambda e: e.activation(out=sd, in_=ss, func=AF.Sqrt, bias=self.epsb[:, 0:1], scale=1.0 / D), reads=[r_ss], writes=[r_ss])
        k.op("dve", lambda e: e.reciprocal(out=rstd, in_=sd), reads=[r_ss], writes=[r_ss])
        r_t = Res("top")
        Lg = k.sb([8, NE], F32); m1 = k.sb([8], F32); m2 = k.sb([8], F32); eq1 = k.sb([8, NE], F32); eq2 = k.sb([8, NE], F32)
        L2 = k.sb([8, NE], F32); dd = k.sb([8], F32); w1 = k.sb([8], F32); w2 = k.sb([8], F32); cmb = k.sb([8, NE], F32); t2 = k.sb([8, NE], F32)

        def bc(a):
            return a.unsqueeze(2).to_broadcast([128, 8, NE])
        k.op("dve", lambda e: e.tensor_tensor(out=Lg, in0=lg, in1=bc(rstd), op=ALU.mult), reads=[r_lg, r_ss], writes=[r_t])
        k.op("dve", lambda e: e.reduce_max(out=m1, in_=Lg, axis=AX.X), reads=[r_t], writes=[r_t])
        k.op("dve", lambda e: e.tensor_tensor(out=eq1, in0=Lg, in1=bc(m1), op=ALU.is_equal), reads=[r_t], writes=[r_t])
        k.op("dve", lambda e: e.scalar_tensor_tensor(out=L2, in0=eq1, scalar=-1e30, in1=Lg, op0=ALU.mult, op1=ALU.add), reads=[r_t], writes=[r_t])
        k.op("dve", lambda e: e.reduce_max(out=m2, in_=L2, axis=AX.X), reads=[r_t], writes=[r_t])
        k.op("dve", lambda e: e.tensor_tensor(out=eq2, in0=L2, in1=bc(m2), op=ALU.is_equal), reads=[r_t], writes=[r_t])
        k.op("dve", lambda e: e.tensor_tensor(out=dd, in0=m1, in1=m2, op=ALU.subtract), reads=[r_t], writes=[r_t])
        k.op("act", lambda e: e.activation(out=w1, in_=dd, func=AF.Sigmoid), reads=[r_t], writes=[r_t])
        k.op("dve", lambda e: e.tensor_scalar(out=w2, in0=w1, scalar1=-1.0, scalar2=1.0, op0=ALU.mult, op1=ALU.add), reads=[r_t], writes=[r_t])
        k.op("dve", lambda e: e.tensor_tensor(out=cmb, in0=eq1, in1=bc(w1), op=ALU.mult), reads=[r_t], writes=[r_t])
        k.op("dve", lambda e: e.tensor_tensor(out=t2, in0=eq2, in1=bc(w2), op=ALU.mult), reads=[r_t], writes=[r_t])
        k.op("dve", lambda e: e.tensor_tensor(out=cmb, in0=cmb, in1=t2, op=ALU.add), reads=[r_t], writes=[r_t])
        cT = k.sb([T], F32); r_cT = Res("cT")
        for tt in range(8):
            b = tt // 4
            k.op("pe", lambda e, tt=tt, b=b: e.transpose(k.ps(b)[0:NE, (tt % 4) * 128:(tt % 4 + 1) * 128], cmb[:, tt, :], self.identf),
                 reads=[r_t], writes=[k.psres[b]], signal=(tt % 4 == 3))
        for b in range(2):
            k.op("act", lambda e, b=b: e.activation(out=cT[0:NE, b * 512:(b + 1) * 512], in_=k.ps(b)[0:NE, :], func=AF.Copy), reads=[k.psres[b]], writes=[r_cT])
        k.dma("sp", cmbT, cT[0:NE, :], reads=[r_cT])
        k.barrier()


_BF = ml_dtypes.bfloat16


_CONSTS = {}


def _host_consts():
    if "c" in _CONSTS:
        return _CONSTS["c"]
    tabs = []
    invs = []
    s_ = np.arange(S, dtype=np.int64)[:, None]
    for q in range(NBLK):
        kk = (q * T + np.arange(T, dtype=np.int64))[None, :]
        ang = 2.0 * np.pi * ((s_ * kk) % S).astype(np.float64) / S
        tabs.append(np.concatenate([np.cos(ang), -np.sin(ang)], axis=1).astype(np.float32).astype(_BF))
        pos = q * T + np.arange(T)
        for w in (2, 4, 8, 16):
            lo = np.clip(pos - w // 2, 0, S - 1)
            hi = np.clip(pos + w // 2 - 1, 0, S - 1)
            invs.append(1.0 / (hi - lo + 1).astype(np.float32))
    dftab = np.concatenate(tabs, axis=0)
    cc = np.arange(256, dtype=np.int64)
    ang2 = 2.0 * np.pi * ((cc[:, None] * cc[None, :]) % 256).astype(np.float64) / 256
    cdft = np.concatenate([np.cos(ang2), np.sin(ang2)], axis=1).astype(np.float32).astype(_BF)
    invcnt = np.concatenate(invs).astype(np.float32)
    _CONSTS["c"] = {"ident": np.eye(128, dtype=np.float32), "dftab": dftab, "cdft": cdft, "invcnt": invcnt}
    return _CONSTS["c"]


def _tile_w(w):
    K, N = w.shape
    return np.ascontiguousarray(w.reshape(K // 128, 128, N // 256, 256).transpose(2, 1, 0, 3)).reshape((N // 256) * 128, (K // 128) * 256)


def _pc(v):
    return np.ascontiguousarray(np.asarray(v, np.float32).reshape(8, 128).T)


def make_in_maps(inputs, names):
    x = np.asarray(inputs["x"], np.float32).reshape(NCORES, NBLK * T, D)
    shared = dict(_host_consts())
    if "final_norm" in names:
        shared["final_norm"] = np.asarray(inputs["final_norm"], np.float32)
    for li in range(2):
        p = "l%d_" % li
        if p + "norm1" not in names:
            continue

        def g(nm):
            return np.asarray(inputs[p + nm], np.float32)
        shared[p + "norm1"] = g("norm1")
        shared[p + "norm2"] = g("norm2")
        shared[p + "conv_wT"] = np.ascontiguousarray(g("conv_w").T)
        shared[p + "chanp"] = np.ascontiguousarray(np.concatenate([_pc(g("conv_b")), _pc(g("conv_ln_g")), _pc(g("conv_ln_b")), _pc(g("pool_scale"))], axis=1))
        shared[p + "pool_w"] = g("pool_w").reshape(4 * 256, 256)
        shared[p + "gmlp_ln_g"] = g("gmlp_ln_g")
        shared[p + "gmlp_ln_b"] = g("gmlp_ln_b")
        shared[p + "gmlp_wsT"] = np.ascontiguousarray(g("gmlp_ws").transpose(2, 0, 1).reshape(128, 8 * 128))
        shared[p + "gmlp_b"] = g("gmlp_b").reshape(8 * 128)
        for nm in ("w_in", "w_gate", "w_out", "ffn_w1", "ffn_w3", "ffn_w2"):
            if p + nm in names:
                shared[p + nm] = _tile_w(g(nm))
        if p + "w_branch" in names:
            shared[p + "w_branch"] = _tile_w(g("w_branch").reshape(4 * BW, D))
        if p + "routerT" in names:
            shared[p + "routerT"] = np.ascontiguousarray(g("router").T).reshape(NE * D)
        for nm in ("exp_w1", "exp_w3", "exp_w2"):
            if p + nm in names:
                shared[p + nm] = np.concatenate([_tile_w(g(nm)[e_]) for e_ in range(NE)], axis=0)
    maps = []
    for c in range(NCORES):
        m = dict(shared)
        m["x"] = x[c]
        maps.append(m)
    return maps


_ALL_INPUTS = (
    "x",
    "l0_norm1",
    "l0_w_in",
    "l0_w_gate",
    "l0_conv_w",
    "l0_conv_b",
    "l0_conv_ln_g",
    "l0_conv_ln_b",
    "l0_pool_w",
    "l0_pool_scale",
    "l0_gmlp_ln_g",
    "l0_gmlp_ln_b",
    "l0_gmlp_ws",
    "l0_gmlp_b",
    "l0_w_branch",
    "l0_w_out",
    "l0_norm2",
    "l0_ffn_w1",
    "l0_ffn_w3",
    "l0_ffn_w2",
    "l1_norm1",
    "l1_w_in",
    "l1_w_gate",
    "l1_conv_w",
    "l1_conv_b",
    "l1_conv_ln_g",
    "l1_conv_ln_b",
    "l1_pool_w",
    "l1_pool_scale",
    "l1_gmlp_ln_g",
    "l1_gmlp_ln_b",
    "l1_gmlp_ws",
    "l1_gmlp_b",
    "l1_w_branch",
    "l1_w_out",
    "l1_norm2",
    "l1_router",
    "l1_exp_w1",
    "l1_exp_w3",
    "l1_exp_w2",
    "final_norm",
)


_CACHE = {}


def kernel(**inputs):
    for nm in _ALL_INPUTS:
        assert nm in inputs, "missing input " + nm
    if "prog" not in _CACHE:
        pr = Prog()
        pr.build()
        _CACHE["prog"] = pr
    pr = _CACHE["prog"]
    maps = make_in_maps(inputs, set(pr.inp.keys()))
    res = run_bass_kernel_spmd(pr.nc, maps, core_ids=list(range(NCORES)))
    out = np.stack([np.asarray(r["out"], np.float32) for r in res.results], axis=0)
    return out.reshape(2, S, D)
```
